# Optimizing a Trainium2 kernel written in Bass

```python
import math
import jax, jax.numpy as jnp
from jax import lax
import numpy as np

D_MODEL = 2048
BATCH = 4
SEQ = 2048
DEPTH = 2

HEAD_DIM = 128
ROT_DIM = HEAD_DIM // 4
ROPE_THETA = 500000.0
A_PATTERNS = ((128, 1), (512, 4), (2048, 16))
A_GROUPS = len(A_PATTERNS)
A_HEADS = 16
B_HEADS = 16
MOBA_BLOCK = 256
MOBA_TOPK = 3
MOBA_QCHUNK = 16
N_EXPERTS = 32
TOP_K = 4
D_FF = 2048
SWIGLU_LIMIT = 7.0
SWIGLU_ALPHA = 1.702
MOE_BLOCK = 128
N_A_LAYERS = DEPTH // 2
N_B_LAYERS = DEPTH - N_A_LAYERS
DEEPNORM_ALPHA = (2 * DEPTH) ** 0.25
DEEPNORM_BETA = (8 * DEPTH) ** -0.25
LN_EPS = 1e-5
NEG_INF = -1e30

kernel_name = 'yoco_dilated_moba_moe_deepnorm'


def layer_norm(x, g, b):
    xf = x.astype(jnp.float32)
    mu = jnp.mean(xf, axis=-1, keepdims=True)
    var = jnp.mean(jnp.square(xf - mu), axis=-1, keepdims=True)
    return ((xf - mu) * lax.rsqrt(var + LN_EPS) * g + b).astype(x.dtype)


def rope_tables(seq):
    inv = ROPE_THETA ** (-jnp.arange(0, ROT_DIM, 2, dtype=jnp.float32) / ROT_DIM)
    ang = jnp.arange(seq, dtype=jnp.float32)[:, None] * inv[None, :]
    return jnp.cos(ang), jnp.sin(ang)


def apply_partial_rope(x, cos, sin):
    half = ROT_DIM // 2
    c = cos[None, :, None, :]
    s = sin[None, :, None, :]
    x1 = x[..., :half].astype(jnp.float32)
    x2 = x[..., half:ROT_DIM].astype(jnp.float32)
    rot = jnp.concatenate([x1 * c - x2 * s, x2 * c + x1 * s], axis=-1).astype(x.dtype)
    return jnp.concatenate([rot, x[..., ROT_DIM:]], axis=-1)


def banded_causal_attention(q, k, v, span):
    n, L, h, dh = q.shape
    blk = span
    pad = (-L) % blk
    nb = (L + pad) // blk
    qb = jnp.pad(q, ((0, 0), (0, pad), (0, 0), (0, 0))).reshape(n, nb, blk, h, dh)

    def key_blocks(t):
        tp = jnp.pad(t, ((0, 0), (blk, pad), (0, 0), (0, 0))).reshape(n, nb + 1, blk, h, dh)
        return jnp.concatenate([tp[:, :-1], tp[:, 1:]], axis=2)

    kb = key_blocks(k)
    vb = key_blocks(v)
    s = jnp.einsum('nbqhd,nbkhd->nbhqk', qb, kb).astype(jnp.float32) * (dh ** -0.5)
    qi = jnp.arange(blk)[:, None]
    ki = jnp.arange(2 * blk)[None, :] - blk
    diff = qi - ki
    band = (diff >= 0) & (diff <= span)
    not_before_start = (jnp.arange(nb)[:, None, None] > 0) | (ki[None] >= 0)
    mask = band[None] & not_before_start
    s = jnp.where(mask[None, :, None], s, NEG_INF)
    m = jnp.max(s, axis=-1, keepdims=True)
    p = jnp.exp(s - m)
    den = jnp.sum(p, axis=-1)
    o = jnp.einsum('nbhqk,nbkhd->nbqhd', p, vb.astype(jnp.float32))
    o = o / jnp.moveaxis(den, -1, 2)[..., None]
    lse = jnp.moveaxis(m[..., 0] + jnp.log(den), -1, 2)
    o = o.reshape(n, nb * blk, h, dh)[:, :L]
    lse = lse.reshape(n, nb * blk, h)[:, :L]
    return o, lse


def dilated_group_attention(q, k, v, window, dilation):
    b, s, h, dh = q.shape
    L = s // dilation

    def to_sub(t):
        return t.reshape(b, L, dilation, h, dh).transpose(0, 2, 1, 3, 4).reshape(b * dilation, L, h, dh)

    o, lse = banded_causal_attention(to_sub(q), to_sub(k), to_sub(v), window // dilation)
    o = o.reshape(b, dilation, L, h, dh).transpose(0, 2, 1, 3, 4).reshape(b, s, h, dh)
    lse = lse.reshape(b, dilation, L, h).transpose(0, 2, 1, 3).reshape(b, s, h)
    return o, lse


def dilated_attention(x, w_qkv, w_o, cos, sin):
    b, s, _ = x.shape
    qkv = (x @ w_qkv).reshape(b, s, A_GROUPS, 3, A_HEADS, HEAD_DIM)
    outs, lses = [], []
    for g, (window, dil) in enumerate(A_PATTERNS):
        q = apply_partial_rope(qkv[:, :, g, 0], cos, sin)
        k = apply_partial_rope(qkv[:, :, g, 1], cos, sin)
        o, lse = dilated_group_attention(q, k, qkv[:, :, g, 2], window, dil)
        outs.append(o)
        lses.append(lse)
    o = jnp.stack(outs)
    wts = jax.nn.softmax(jnp.stack(lses), axis=0)
    mixed = jnp.sum(wts[..., None] * o, axis=0).astype(x.dtype)
    return mixed.reshape(b, s, A_HEADS * HEAD_DIM) @ w_o


def shared_kv(h, w_kv, cos, sin):
    b, s, _ = h.shape
    kv = (h @ w_kv).reshape(b, s, 2, B_HEADS, HEAD_DIM)
    k = apply_partial_rope(kv[:, :, 0], cos, sin)
    v = kv[:, :, 1]
    nblk = -(-s // MOBA_BLOCK)
    pad = nblk * MOBA_BLOCK - s

    def blocks(t):
        t = jnp.pad(t, ((0, 0), (0, pad), (0, 0), (0, 0)))
        return t.reshape(b, nblk, MOBA_BLOCK, B_HEADS, HEAD_DIM).transpose(0, 3, 1, 2, 4)

    kb = blocks(k)
    vb = blocks(v)
    k_mean = jnp.mean(kb.astype(jnp.float32), axis=3).astype(kb.dtype)
    return kb, vb, k_mean


def moba_attention(x, w_q, w_o, kb, vb, k_mean, cos, sin):
    b, s, _ = x.shape
    nblk = kb.shape[2]
    topk = min(MOBA_TOPK, nblk)
    scale = HEAD_DIM ** -0.5
    q = apply_partial_rope((x @ w_q).reshape(b, s, B_HEADS, HEAD_DIM), cos, sin).transpose(0, 2, 1, 3)
    bi = jnp.arange(b)[:, None, None, None]
    hi = jnp.arange(B_HEADS)[None, :, None, None]

    def one_chunk(ci):
        start = ci * MOBA_QCHUNK
        qc = lax.dynamic_slice_in_dim(q, start, MOBA_QCHUNK, axis=2)
        cur = start // MOBA_BLOCK
        gate = jnp.einsum('bhqd,bhnd->bhqn', qc, k_mean).astype(jnp.float32)
        gate = jnp.where(jnp.arange(nblk) < cur, gate, NEG_INF)
        _, sel = lax.top_k(gate, topk)
        k_sel = kb[bi, hi, sel]
        v_sel = vb[bi, hi, sel]
        s_sel = jnp.einsum('bhqd,bhqnkd->bhqnk', qc, k_sel).astype(jnp.float32) * scale
        s_sel = jnp.where((jnp.arange(topk) < cur)[:, None], s_sel, NEG_INF)
        k_own = lax.dynamic_index_in_dim(kb, cur, axis=2, keepdims=False)
        v_own = lax.dynamic_index_in_dim(vb, cur, axis=2, keepdims=False)
        s_own = jnp.einsum('bhqd,bhkd->bhqk', qc, k_own).astype(jnp.float32) * scale
        kpos = cur * MOBA_BLOCK + jnp.arange(MOBA_BLOCK)
        qpos = start + jnp.arange(MOBA_QCHUNK)
        s_own = jnp.where(kpos[None, :] <= qpos[:, None], s_own, NEG_INF)
        scores = jnp.concatenate([s_sel.reshape(b, B_HEADS, MOBA_QCHUNK, topk * MOBA_BLOCK), s_own], axis=-1)
        p = jax.nn.softmax(scores, axis=-1)
        p_sel = p[..., :topk * MOBA_BLOCK].reshape(b, B_HEADS, MOBA_QCHUNK, topk, MOBA_BLOCK)
        p_own = p[..., topk * MOBA_BLOCK:]
        o = (jnp.einsum('bhqnk,bhqnkd->bhqd', p_sel, v_sel.astype(jnp.float32))
             + jnp.einsum('bhqk,bhkd->bhqd', p_own, v_own.astype(jnp.float32)))
        return o.astype(x.dtype)

    o = lax.map(one_chunk, jnp.arange(s // MOBA_QCHUNK, dtype=jnp.int32))
    o = o.transpose(1, 0, 3, 2, 4).reshape(b, s, B_HEADS * HEAD_DIM)
    return o @ w_o


def moe_ffn(x, w_router, b_router, w_gu, b_gu, w_down, b_down):
    b, s, d = x.shape
    t = x.reshape(-1, d)
    n_tok = t.shape[0]
    logits = (t @ w_router + b_router).astype(jnp.float32)
    top_val, top_idx = lax.top_k(logits, TOP_K)
    top_w = jax.nn.softmax(top_val, axis=-1)
    n_slots = n_tok * TOP_K
    slot_e = top_idx.reshape(-1).astype(jnp.int32)
    slot_tok = jnp.repeat(jnp.arange(n_tok, dtype=jnp.int32), TOP_K)
    slot_w = top_w.reshape(-1)
    order = jnp.argsort(slot_e)
    e_sorted = slot_e[order]
    tok_sorted = slot_tok[order]
    w_sorted = slot_w[order]
    counts = jnp.zeros(N_EXPERTS, jnp.int32).at[slot_e].add(1)
    group_start = jnp.cumsum(counts) - counts
    padded = (counts + MOE_BLOCK - 1) // MOE_BLOCK * MOE_BLOCK
    padded_end = jnp.cumsum(padded)
    padded_start = padded_end - padded
    dest = padded_start[e_sorted] + jnp.arange(n_slots, dtype=jnp.int32) - group_start[e_sorted]
    cap = (n_slots + N_EXPERTS * MOE_BLOCK + MOE_BLOCK - 1) // MOE_BLOCK * MOE_BLOCK
    buf_tok = jnp.zeros(cap, jnp.int32).at[dest].set(tok_sorted)
    buf_w = jnp.zeros(cap, jnp.float32).at[dest].set(w_sorted)
    n_blocks = cap // MOE_BLOCK
    block_e = jnp.searchsorted(padded_end, jnp.arange(n_blocks, dtype=jnp.int32) * MOE_BLOCK, side='right')
    block_e = jnp.minimum(block_e, N_EXPERTS - 1).astype(jnp.int32)

    def expert_block(args):
        e, tok = args
        gu = t[tok] @ w_gu[e] + b_gu[e]
        gate = jnp.minimum(gu[:, :D_FF], SWIGLU_LIMIT)
        lin = jnp.clip(gu[:, D_FF:], -SWIGLU_LIMIT, SWIGLU_LIMIT)
        hid = (lin + 1.0) * gate * jax.nn.sigmoid(SWIGLU_ALPHA * gate)
        return hid @ w_down[e] + b_down[e]

    y = lax.map(expert_block, (block_e, buf_tok.reshape(n_blocks, MOE_BLOCK)))
    y = y.reshape(cap, d).astype(jnp.float32) * buf_w[:, None]
    out = jnp.zeros((n_tok, d), jnp.float32).at[buf_tok].add(y)
    return out.astype(x.dtype).reshape(b, s, d)


def setup_inputs(seed: int = 0) -> dict:
    key = jax.random.key(seed)
    ks = jax.random.split(key, 18)
    f32 = jnp.float32
    d = D_MODEL
    a_width = A_HEADS * HEAD_DIM
    b_width = B_HEADS * HEAD_DIM
    x = jax.random.normal(ks[0], (BATCH, SEQ, d), f32)
    qkv_scale = jnp.array([1.0, 1.0, DEEPNORM_BETA], f32)[:, None, None]
    a_w_qkv = (jax.random.normal(ks[1], (N_A_LAYERS, d, A_GROUPS, 3, A_HEADS, HEAD_DIM), f32)
               * (d ** -0.5) * qkv_scale).reshape(N_A_LAYERS, d, A_GROUPS * 3 * a_width)
    a_w_o = jax.random.normal(ks[2], (N_A_LAYERS, a_width, d), f32) * (a_width ** -0.5) * DEEPNORM_BETA
    kv_scale = jnp.array([1.0, DEEPNORM_BETA], f32)[:, None, None]
    kv_w = (jax.random.normal(ks[3], (d, 2, B_HEADS, HEAD_DIM), f32) * (d ** -0.5) * kv_scale).reshape(d, 2 * b_width)
    b_w_q = jax.random.normal(ks[4], (N_B_LAYERS, d, b_width), f32) * (d ** -0.5)
    b_w_o = jax.random.normal(ks[5], (N_B_LAYERS, b_width, d), f32) * (b_width ** -0.5) * DEEPNORM_BETA
    router_w = jax.random.normal(ks[6], (DEPTH, d, N_EXPERTS), f32) * (d ** -0.5)
    router_b = jax.random.normal(ks[7], (DEPTH, N_EXPERTS), f32) * 0.01
    moe_w_gate_up = jax.random.normal(ks[8], (DEPTH, N_EXPERTS, d, 2 * D_FF), f32) * (d ** -0.5)
    moe_b_gate_up = jax.random.normal(ks[9], (DEPTH, N_EXPERTS, 2 * D_FF), f32) * 0.01
    moe_w_down = jax.random.normal(ks[10], (DEPTH, N_EXPERTS, D_FF, d), f32) * (D_FF ** -0.5) * DEEPNORM_BETA
    moe_b_down = jax.random.normal(ks[11], (DEPTH, N_EXPERTS, d), f32) * 0.01
    ln1_g = 1.0 + 0.02 * jax.random.normal(ks[12], (DEPTH, d), f32)
    ln1_b = 0.02 * jax.random.normal(ks[13], (DEPTH, d), f32)
    ln2_g = 1.0 + 0.02 * jax.random.normal(ks[14], (DEPTH, d), f32)
    ln2_b = 0.02 * jax.random.normal(ks[15], (DEPTH, d), f32)
    return {'x': x, 'a_w_qkv': a_w_qkv, 'a_w_o': a_w_o, 'kv_w': kv_w, 'b_w_q': b_w_q, 'b_w_o': b_w_o,
            'router_w': router_w, 'router_b': router_b, 'moe_w_gate_up': moe_w_gate_up,
            'moe_b_gate_up': moe_b_gate_up, 'moe_w_down': moe_w_down, 'moe_b_down': moe_b_down,
            'ln1_g': ln1_g, 'ln1_b': ln1_b, 'ln2_g': ln2_g, 'ln2_b': ln2_b}


def reference(x, a_w_qkv, a_w_o, kv_w, b_w_q, b_w_o, router_w, router_b, moe_w_gate_up,
              moe_b_gate_up, moe_w_down, moe_b_down, ln1_g, ln1_b, ln2_g, ln2_b):
    s = x.shape[1]
    cos, sin = rope_tables(s)
    h = x
    kb = vb = k_mean = None
    for layer in range(DEPTH):
        if layer < N_A_LAYERS:
            mix = dilated_attention(h, a_w_qkv[layer], a_w_o[layer], cos, sin)
        else:
            if layer == N_A_LAYERS:
                kb, vb, k_mean = shared_kv(h, kv_w, cos, sin)
            j = layer - N_A_LAYERS
            mix = moba_attention(h, b_w_q[j], b_w_o[j], kb, vb, k_mean, cos, sin)
        h = layer_norm(DEEPNORM_ALPHA * h + mix, ln1_g[layer], ln1_b[layer])
        ffn = moe_ffn(h, router_w[layer], router_b[layer], moe_w_gate_up[layer], moe_b_gate_up[layer],
                      moe_w_down[layer], moe_b_down[layer])
        h = layer_norm(DEEPNORM_ALPHA * h + ffn, ln2_g[layer], ln2_b[layer])
    return h
```

```python
import numpy as np
import ml_dtypes
from contextlib import ExitStack
import concourse.bass as bass
import concourse.mybir as mybir
from concourse.bass_utils import run_bass_kernel_spmd

F32 = mybir.dt.float32
BF16 = mybir.dt.bfloat16
AF = mybir.ActivationFunctionType
ALU = mybir.AluOpType
AX = mybir.AxisListType

SEM_EPOCH = 30000
D = 2048
T = 1024
NE = 32
CAP = 256
ALPHA = 4.0 ** 0.25
EPS = 1e-5
SCALE = 128.0 ** -0.5
SW_ALPHA = 1.702
SW_LIM = 7.0
NEG = -30000.0


def sl(start, count, step):
    return slice(start, start + (count - 1) * step + 1, step)


class Buf:
    __slots__ = ("name", "last_w", "readers", "excl")

    def __init__(self, name="", excl=False):
        self.name = name
        self.last_w = None
        self.readers = []
        self.excl = excl


class Sched:
    ENGS = ("pe", "act", "dve", "pool", "sp")

    def __init__(self, nc):
        self.nc = nc
        self.ops = {e: [] for e in self.ENGS}
        self.cnt = {e: 0 for e in self.ENGS}
        self.sem = {e: nc.alloc_semaphore(name=f"s_{e}_0") for e in self.ENGS}
        self.epoch = {e: 0 for e in self.ENGS}
        self.seen = {e: {} for e in self.ENGS}
        self.dma_sems = {}
        self.free_dma = []
        self.n_dma_sem = 0
        self.all_dma = []
        self.old_sems = []
        self.n_inst = {e: 0 for e in self.ENGS}

    def _wait(self, eng, dep):
        sem, val = dep
        key = id(sem)
        if self.seen[eng].get(key, 0) >= val:
            return
        self.seen[eng][key] = val
        self.ops[eng].append(("wait", sem, val))

    def _deps(self, eng, reads, writes):
        deps = []
        for b in reads:
            if b.last_w is not None:
                deps.append(b.last_w)
            if b.excl:
                deps.extend(b.readers)
        for b in writes:
            if b.last_w is not None:
                deps.append(b.last_w)
            deps.extend(b.readers)
        for d in deps:
            if eng == "pe" and d[0] is self.sem["pe"]:
                continue
            self._wait(eng, d)

    def _record(self, produced, reads, writes):
        for b in writes:
            b.last_w = produced
            b.readers = []
        for b in reads:
            if b.last_w is produced:
                continue
            b.readers.append(produced)

    def _next(self, eng):
        if self.cnt[eng] >= SEM_EPOCH:
            self.old_sems.append((self.sem[eng], self.cnt[eng]))
            self.epoch[eng] += 1
            self.sem[eng] = self.nc.alloc_semaphore(name=f"s_{eng}_{self.epoch[eng]}")
            self.cnt[eng] = 0
        self.cnt[eng] += 1
        return (self.sem[eng], self.cnt[eng])

    def op(self, eng, fn, reads=(), writes=()):
        self._deps(eng, reads, writes)
        produced = self._next(eng)
        self.ops[eng].append(("op", fn, produced[0], 1))
        self._record(produced, reads, writes)
        self.n_inst[eng] += 1

    def group(self, eng, fns, reads=(), writes=()):
        self._deps(eng, reads, writes)
        for fn in fns[:-1]:
            self.ops[eng].append(("op", fn, None, 0))
        produced = self._next(eng)
        self.ops[eng].append(("op", fns[-1], produced[0], 1))
        self._record(produced, reads, writes)
        self.n_inst[eng] += len(fns)

    def dma(self, eng, fn, semname, reads=(), writes=()):
        self._deps(eng, reads, writes)
        if writes:
            semname = f"{semname}_{writes[0].name}_{id(writes[0])}"
        ent = self.dma_sems.get(semname)
        if ent is None and self.free_dma:
            ent = self.free_dma.pop()
            self.dma_sems[semname] = ent
        if ent is None or ent[1] + 16 > SEM_EPOCH:
            if ent is not None:
                self.old_sems.append((ent[0], ent[1]))
            idx = 0 if ent is None else ent[2] + 1
            self.n_dma_sem += 1
            ent = [self.nc.alloc_semaphore(name=f"d{self.n_dma_sem}_{idx}"), 0, idx]
            self.dma_sems[semname] = ent
        ent[1] += 16
        produced = (ent[0], ent[1])
        self.ops[eng].append(("op", fn, ent[0], 16))
        self._record(produced, reads, writes)
        self.n_inst[eng] += 1
        return produced

    def barrier(self):
        deps = []
        for e in self.ENGS:
            if self.cnt[e] > 0:
                deps.append((self.sem[e], self.cnt[e]))
        for ent in self.dma_sems.values():
            deps.append((ent[0], ent[1]))
        deps.extend(self.old_sems)
        for e in self.ENGS:
            for d in deps:
                if d[0] is self.sem[e]:
                    continue
                self._wait(e, d)
        for ent in self.dma_sems.values():
            if not any(ent is x for x in self.all_dma):
                self.all_dma.append(ent)
            self.free_dma.append(ent)
        self.dma_sems = {}

    def emit(self, final_deps):
        for d in final_deps:
            self._wait("sp", d)
        ops = self.ops

        def run(e, lst):
            for it in lst:
                if it[0] == "wait":
                    e.wait_ge(it[1], it[2])
                else:
                    ins = it[1](e)
                    if it[2] is not None:
                        ins.then_inc(it[2], it[3])

        sems = ([h for h in self.sem.values()] + [h for (h, _) in self.old_sems] + [ent[0] for ent in self.dma_sems.values()]
                + [ent[0] for ent in self.all_dma] + [ent[0] for ent in self.free_dma])
        uniq = []
        for h in sems:
            if not any(h is u for u in uniq):
                uniq.append(h)
        self.nc.all_engine_barrier()
        for h in uniq:
            self.nc.gpsimd.sem_clear(h)
        self.nc.all_engine_barrier()
        with self.nc.Block() as block:
            @block.tensor
            def _(e):
                run(e, ops["pe"])

            @block.scalar
            def _(e):
                run(e, ops["act"])

            @block.vector
            def _(e):
                run(e, ops["dve"])

            @block.gpsimd
            def _(e):
                run(e, ops["pool"])

            @block.sync
            def _(e):
                run(e, ops["sp"])
        self.nc.all_engine_barrier()
        for h in uniq:
            self.nc.gpsimd.sem_clear(h)
        self.nc.all_engine_barrier()


CBA_W = 32 + 128 + 512 * 4 + 1024
CBM_W = 128 + 128 + 4096
CFM_W = 256 + 2


def host_consts(hf):
    pv = 1.0 if hf == 1 else 0.0
    k = np.arange(128)[:, None]
    q = np.arange(128)[None, :]
    A = (k <= q).astype(np.float32)
    B = (k >= q).astype(np.float32)
    cba = np.zeros((128, CBA_W), np.float32)
    o = 0
    perm = np.zeros((32, 32), np.float32)
    for m in range(32):
        perm[(m + 16) % 32, m] = 1.0
    cba[0:32, o:o + 32] = perm
    o += 32
    cba[:, o:o + 128] = 1.0
    o += 128
    cba[:, o:o + 512] = np.concatenate([B * pv, A, B, A], axis=1)
    o += 512
    cba[:, o:o + 512] = np.concatenate([B, A, B, A], axis=1)
    o += 512
    q64 = np.arange(64)[None, :]
    m2 = ((k <= 64 + q64) & ((k >= 64) | (pv > 0))).astype(np.float32)
    cba[:, o:o + 512] = np.tile(m2, (1, 8))
    o += 512
    q256 = np.arange(256)[None, :]
    cba[:, o:o + 512] = np.concatenate([(k <= q256), (128 + k <= q256)], axis=1).astype(np.float32)
    o += 512
    for n in range(8):
        cba[n, o + n * 128:o + (n + 1) * 128] = 1.0
    o += 1024
    assert o == CBA_W
    cbm = np.zeros((128, CBM_W), np.float32)
    cbm[:, 0:128] = 1.0
    cbm[:, 128:256] = (k < q).astype(np.float32)
    for e in range(32):
        cbm[e, 256 + e * 128:256 + (e + 1) * 128] = 1.0
    cfm = np.zeros((128, CFM_W), np.float32)
    cfm[:, 0:256] = np.arange(256, dtype=np.float32)[None, :]
    cfm[:, 256] = np.arange(128)
    cfm[:, 257] = np.arange(128) + 128
    gb = np.zeros((128, 8, 8), np.float32)
    for qt in range(8):
        cur = 4 + qt // 2
        for n in range(8):
            if n >= cur or (n < 4 and pv == 0.0):
                gb[:, qt, n] = NEG
    pos = np.arange(2048, dtype=np.float32)
    if hf == 0:
        pos = np.maximum(pos - 1024.0, 0.0).astype(np.float32)
    inv = (np.float32(500000.0) ** (-(np.arange(0, 32, 2, dtype=np.float32)) / np.float32(32.0))).astype(np.float32)
    ang = (pos[None, :] * inv[:, None]).astype(np.float32)
    c = np.cos(ang).astype(np.float32)
    s = np.sin(ang).astype(np.float32)
    rc = np.concatenate([c, c], axis=0)
    rs = np.concatenate([-s, s], axis=0)
    return dict(cba=cba.astype(ml_dtypes.bfloat16), cbm=cbm.astype(ml_dtypes.bfloat16), cfm=cfm,
                gbias=gb.reshape(128, 64), rc=np.ascontiguousarray(rc), rs=np.ascontiguousarray(rs))


def build_program(layers, n_exp=NE, dbg=None, part=None):
    nc = bass.Bass("TRN2", target_bir_lowering=False)
    S = Sched(nc)

    def din(name, shape, dt=F32):
        return nc.dram_tensor(name, list(shape), dt, kind="ExternalInput").ap()

    xo = din("xo", [T, D])
    xp = din("xp", [T, D])
    W = {}
    if 0 in layers and part != "C":
        W["a_w_qkv"] = din("a_w_qkv", [D, 18432])
        W["a_w_o"] = din("a_w_o", [D, D])
    if 1 in layers and part != "C":
        W["kv_w"] = din("kv_w", [D, 4096])
        W["b_w_q"] = din("b_w_q", [D, D])
        W["b_w_o"] = din("b_w_o", [D, D])
    lgin = din("lgin", [T, NE]) if part == "C" else None
    W["router_w"] = din("router_w", [2, D, NE])
    W["router_b"] = din("router_b", [2, NE])
    import os as _os3
    if part != "AB" and (_os3.environ.get("FORCE_MOE") or not ((dbg or "").startswith("mid") or dbg in ("acc0", "lg"))):
        for L_ in layers:
            W[f"w_gu{L_}"] = din(f"w_gu{L_}", [max(n_exp, 1), D, 4096])
            W[f"w_dn{L_}"] = din(f"w_dn{L_}", [max(n_exp, 1), D, D])
        W["b_gu"] = din("moe_b_gate_up", [2, NE, 4096])
        W["b_dn"] = din("moe_b_down", [2, NE, D])
    for nm in ("ln1_g", "ln1_b", "ln2_g", "ln2_b"):
        W[nm] = din(nm, [2, D])
    c_cba = din("cba", [128, CBA_W], BF16)
    c_cbm = din("cbm", [128, CBM_W], BF16)
    c_cfm = din("cfm", [128, CFM_W])
    c_gb = din("gbias", [128, 64])
    c_rc = din("rc", [32, 2048])
    c_rs = din("rs", [32, 2048])
    out = nc.dram_tensor("out", [T, D], F32, kind="ExternalOutput").ap()
    lgout = nc.dram_tensor("lgout", [T, NE], F32, kind="ExternalOutput").ap() if part == "AB" else None
    dbg_out = None
    if dbg is not None:
        dbg_out = nc.dram_tensor("dbg", [T, D], F32, kind="ExternalOutput").ap()
    hmid = None
    if len(layers) == 2:
        hmid = nc.dram_tensor("hmid", [T, D], F32, kind="Internal").ap()

    identF = nc.alloc_sbuf_tensor("identF", [128, 128], F32)
    b_ident = Buf("ident")
    S.op("pool", lambda e: e.memset(identF[:], 0.0), writes=[b_ident])
    S.op("pool", lambda e: e.affine_select(out=identF[:], in_=identF[:], pattern=[[-1, 128]],
                                           compare_op=ALU.not_equal, fill=1.0, base=0, channel_multiplier=1),
         reads=[b_ident], writes=[b_ident])
    psb = [nc.alloc_psum_tensor(f"ps{i}", [128, 512], F32) for i in range(8)]
    b_ps = [Buf(f"ps{i}", excl=True) for i in range(8)]
    bank_ctr = [0]

    avoid_banks = []

    def nb():
        while True:
            i = bank_ctr[0] % 8
            bank_ctr[0] += 1
            if not any(psb[i] is a for a in avoid_banks):
                return psb[i], b_ps[i]

    final_deps = []

    ones_row = nc.alloc_sbuf_tensor("ones_row", [1, 128], F32)
    b_onesrow = Buf("ones_row")
    S.op("dve", lambda e: e.memset(ones_row[:], 1.0), writes=[b_onesrow])

    def bcast_row(dst_t, b_dst, src_row_ap, n, row_t, b_row):
        S.dma("sp", lambda e: e.dma_start(out=row_t[0:1, 0:n], in_=src_row_ap), "brow", writes=[b_row])
        for c0 in range(0, n, 512):
            c1 = min(n, c0 + 512)
            bank, bb = nb()
            S.op("pe", lambda e, bank=bank, c0=c0, c1=c1: e.matmul(bank[:, 0:c1 - c0], lhsT=ones_row[0:1, :], rhs=row_t[0:1, c0:c1],
                                                                    start=True, stop=True), reads=[b_row, b_onesrow], writes=[bb])
            S.op("act", lambda e, bank=bank, c0=c0, c1=c1: e.copy(out=dst_t[:, c0:c1], in_=bank[:, 0:c1 - c0]),
                 reads=[bb], writes=[b_dst])

    def emit_ln(zs, bz, st, mv, rstd, b_st, g_bc, be_bc, b_gbc):
        zf = zs(0, D)
        for c in range(4):
            S.op("dve", lambda e, c=c: e.bn_stats(out=st[:, c, :], in_=zs(c * 512, (c + 1) * 512)), reads=[bz], writes=[b_st])
        S.op("dve", lambda e: e.bn_aggr(out=mv[:], in_=st[:].rearrange("p c s -> p (c s)")), reads=[b_st], writes=[b_st])
        S.op("dve", lambda e: e.tensor_scalar(out=rstd[:], in0=mv[:, 1:2], scalar1=EPS, scalar2=None, op0=ALU.add),
             reads=[b_st], writes=[b_st])
        S.op("act", lambda e: e.activation(out=rstd[:], in_=rstd[:], func=AF.Ln), reads=[b_st], writes=[b_st])
        S.op("act", lambda e: e.activation(out=rstd[:], in_=rstd[:], func=AF.Exp, scale=-0.5), reads=[b_st], writes=[b_st])
        S.op("dve", lambda e: e.tensor_scalar(out=zf, in0=zf, scalar1=mv[:, 0:1], scalar2=rstd[:, 0:1],
                                              op0=ALU.subtract, op1=ALU.mult), reads=[bz, b_st], writes=[bz])
        S.op("pool", lambda e: e.tensor_tensor(out=zf, in0=zf, in1=g_bc[:], op=ALU.mult), reads=[bz, b_gbc], writes=[bz])
        S.op("dve", lambda e: e.tensor_tensor(out=zf, in0=zf, in1=be_bc[:], op=ALU.add), reads=[bz, b_gbc], writes=[bz])

    def run_layer(L, x_own, x_prev, dst, dst_is_out):
        top_lg = ExitStack()
        top_mix = ExitStack()
        LG = top_lg.enter_context(nc.sbuf_tensor(f"L{L}_LG", [128, 8, NE], F32))
        b_LG = Buf("LG")
        _pad = top_lg.enter_context(nc.sbuf_tensor(f"L{L}_pad", [128, 256], F32))
        mixT = top_mix.enter_context(nc.sbuf_tensor(f"L{L}_mixT", [128, 16, T], BF16, side="right"))
        b_mixT = Buf("mixT")
        def phases_AB():
            with ExitStack() as es:
                def A(name, shape, dt):
                    return es.enter_context(nc.sbuf_tensor(f"L{L}_{name}", list(shape), dt))

                XT = A("XT", [128, 16, 2048], BF16)
                b_XT = Buf("XT")
                cba = A("cba", [128, CBA_W], BF16)
                b_cba = Buf("cba")
                rc = A("rc", [32, 2048], F32)
                rs = A("rs", [32, 2048], F32)
                b_rope = Buf("rope")
                S.dma("sp", lambda e: e.dma_start(out=cba[:], in_=c_cba), "const", writes=[b_cba])
                S.dma("sp", lambda e: e.dma_start(out=rc[:], in_=c_rc), "const", writes=[b_rope])
                S.dma("sp", lambda e: e.dma_start(out=rs[:], in_=c_rs), "const", writes=[b_rope])
                perm32 = cba[0:32, 0:32]
                ones_b = cba[:, 32:160]
                MK01 = cba[:, 160:672]
                MK = cba[:, 672:1184]
                MK2 = cba[:, 1184:1696]
                CM = cba[:, 1696:2208]
                E8 = cba[0:32, 2208:3232]


                with ExitStack() as es1:
                    xs = [es1.enter_context(nc.sbuf_tensor(f"L{L}_xs{i}", [128, D], F32)) for i in range(2)]
                    b_xs = [Buf("xs0"), Buf("xs1")]
                    for t in range(16):
                        src = x_prev[t * 128:(t + 1) * 128, :] if t < 8 else x_own[(t - 8) * 128:(t - 7) * 128, :]
                        xi = xs[t % 2]
                        bx = b_xs[t % 2]
                        S.dma("sp", lambda e, xi=xi, src=src: e.dma_start(out=xi[:], in_=src), "xs", writes=[bx])
                        for qd in range(4):
                            bank, bb = nb()
                            S.group("pe", [
                                (lambda e, bank=bank, xi=xi, k=qd * 4 + kk, kk=kk:
                                 e.transpose(bank[:, kk * 128:(kk + 1) * 128], xi[:, k * 128:(k + 1) * 128], identF[:]))
                                for kk in range(4)], reads=[bx, b_ident], writes=[bb])
                            dst_ap = XT[:, qd * 4:(qd + 1) * 4, t * 128:(t + 1) * 128]
                            src_ap = bank[:].rearrange("p (k t) -> p k t", k=4)
                            if (t * 4 + qd) % 2 == 0:
                                S.op("act", lambda e, d=dst_ap, s=src_ap: e.copy(out=d, in_=s), reads=[bb], writes=[b_XT])
                            else:
                                S.op("dve", lambda e, d=dst_ap, s=src_ap: e.tensor_copy(out=d, in_=s), reads=[bb], writes=[b_XT])
                    S.barrier()

                with ExitStack() as es2:
                    def A2(name, shape, dt):
                        return es2.enter_context(nc.sbuf_tensor(f"L{L}_{name}", list(shape), dt))

                    wu = [A2(f"wu{i}", [128, 16, 128], BF16) for i in range(6)]
                    b_wu = [Buf(f"wu{i}") for i in range(6)]
                    QT = A2("QT", [128, T], BF16)
                    KT = A2("KT", [128, 2048], BF16)
                    V = A2("V", [128, 16, 128], BF16)
                    b_QT, b_KT, b_V = Buf("QT"), Buf("KT"), Buf("V")
                    accn = A2("accn", [128, T], F32)
                    accd = A2("accd", [128, T], F32)
                    b_acc = Buf("acc")
                    PT = [A2(f"PT{i}", [128, 512], BF16) for i in range(2)]
                    b_PT = [Buf("PT0"), Buf("PT1")]
                    t1 = A2("rt1", [32, 512], F32)
                    t2 = A2("rt2", [32, 512], F32)
                    b_t1, b_t2 = Buf("t1"), Buf("t2")
                    wu_ctr = [0]
                    pt_ctr = [0]
                    for tz, bz_ in ((QT, b_QT), (KT, b_KT), (V, b_V), (accn, b_acc), (accd, b_acc), (PT[0], b_PT[0]), (PT[1], b_PT[1]),
                                    (t1, b_t1), (t2, b_t2)):
                        S.op("dve", lambda e, tz=tz: e.memset(tz[:], 0.0), writes=[bz_])
                    for i in range(6):
                        S.op("dve", lambda e, i=i: e.memset(wu[i][:], 0.0), writes=[b_wu[i]])

                    def load_w(src_ap):
                        i = wu_ctr[0] % 6
                        wu_ctr[0] += 1
                        S.dma("pool", lambda e, i=i, src_ap=src_ap: e.dma_start(out=wu[i][:], in_=src_ap), f"wu{i}",
                              writes=[b_wu[i]])
                        return wu[i], b_wu[i]

                    def wview(w_ap, c0):
                        return w_ap.rearrange("(k p) f -> p k f", p=128)[:, :, c0:c0 + 128]

                    def proj_T(w, bw, dstT, b_dst, runs):
                        for (ps, N, o0) in runs:
                            bank, bb = nb()
                            S.group("pe", [
                                (lambda e, bank=bank, k=k, ps=ps, N=N: e.matmul(bank[:, 0:N], lhsT=w[:, k, :], rhs=XT[:, k, ps],
                                                                              start=(k == 0), stop=(k == 15)))
                                for k in range(16)], reads=[bw, b_XT], writes=[bb])
                            S.op("act", lambda e, bank=bank, N=N, o0=o0: e.copy(out=dstT[:, o0:o0 + N], in_=bank[:, 0:N]),
                                 reads=[bb], writes=[b_dst])
                            bank2, bb2 = nb()
                            S.op("pe", lambda e, bank2=bank2, N=N, o0=o0: e.matmul(bank2[0:32, 0:N], lhsT=perm32,
                                                                                  rhs=dstT[0:32, o0:o0 + N], start=True, stop=True),
                                 reads=[b_dst, b_cba], writes=[bb2])
                            S.op("dve", lambda e, bank=bank, N=N, ps=ps: e.tensor_tensor(out=t1[:, 0:N], in0=bank[0:32, 0:N],
                                                                                         in1=rc[:, ps], op=ALU.mult),
                                 reads=[bb, b_rope], writes=[b_t1])
                            S.op("dve", lambda e, bank2=bank2, N=N, ps=ps: e.tensor_tensor(out=t2[:, 0:N], in0=bank2[0:32, 0:N],
                                                                                           in1=rs[:, ps], op=ALU.mult),
                                 reads=[bb2, b_rope], writes=[b_t2])
                            S.op("dve", lambda e, N=N, o0=o0: e.tensor_tensor(out=dstT[0:32, o0:o0 + N], in0=t1[:, 0:N],
                                                                              in1=t2[:, 0:N], op=ALU.add),
                                 reads=[b_t1, b_t2], writes=[b_dst])

                    def proj_V(w, bw, blocks):
                        for b0 in range(0, len(blocks), 4):
                            bank, bb = nb()
                            fns = []
                            chunk = blocks[b0:b0 + 4]
                            for j, ps in enumerate(chunk):
                                for k in range(16):
                                    fns.append(lambda e, bank=bank, j=j, ps=ps, k=k: e.matmul(
                                        bank[:, j * 128:(j + 1) * 128], lhsT=XT[:, k, ps], rhs=w[:, k, :],
                                        start=(k == 0), stop=(k == 15)))
                            S.group("pe", fns, reads=[bw, b_XT], writes=[bb])
                            n = len(chunk)
                            S.op("act", lambda e, bank=bank, b0=b0, n=n: e.copy(
                                out=V[:, b0:b0 + n, :], in_=bank[:, 0:n * 128].rearrange("p (j c) -> p j c", c=128)),
                                reads=[bb], writes=[b_V])

                    def att_bank(qk_list, mask_ap, pv_list, Ob, bO, Db, bD):
                        bank, bb = nb()
                        fns = []
                        rd = [b_KT, b_QT]
                        for (c0, N, kcols, qcols, extra) in qk_list:
                            if extra is None:
                                fns.append(lambda e, bank=bank, c0=c0, N=N, kcols=kcols, qcols=qcols: e.matmul(
                                    bank[:, c0:c0 + N], lhsT=KT[:, kcols], rhs=QT[:, qcols], start=True, stop=True))
                            else:
                                el, er = extra
                                fns.append(lambda e, bank=bank, c0=c0, N=N, kcols=kcols, qcols=qcols: e.matmul(
                                    bank[:, c0:c0 + N], lhsT=KT[:, kcols], rhs=QT[:, qcols], start=True, stop=False))
                                fns.append(lambda e, bank=bank, c0=c0, N=N, el=el, er=er: e.matmul(
                                    bank[:, c0:c0 + N], lhsT=el, rhs=er, start=False, stop=True))
                        S.group("pe", fns, reads=rd + [b_cba, b_selT], writes=[bb])
                        pi = pt_ctr[0] % 2
                        pt_ctr[0] += 1
                        P, bP = PT[pi], b_PT[pi]
                        S.op("act", lambda e, bank=bank, P=P: e.activation(out=P[:], in_=bank[:], func=AF.Exp, scale=SCALE),
                             reads=[bb], writes=[bP])
                        if mask_ap is not None:
                            S.op("dve", lambda e, P=P, m=mask_ap: e.tensor_tensor(out=P[:], in0=P[:], in1=m, op=ALU.mult),
                                 reads=[bP, b_cba], writes=[bP])
                        fo, fd = [], []
                        for (vi, pc, N, oc, st, sp) in pv_list:
                            fo.append(lambda e, vi=vi, pc=pc, N=N, oc=oc, st=st, sp=sp, P=P: e.matmul(
                                Ob[:, oc:oc + N], lhsT=V[:, vi, :], rhs=P[:, pc:pc + N], start=st, stop=sp))
                            fd.append(lambda e, pc=pc, N=N, oc=oc, st=st, sp=sp, P=P: e.matmul(
                                Db[:, oc:oc + N], lhsT=ones_b, rhs=P[:, pc:pc + N], start=st, stop=sp))
                        S.group("pe", fo, reads=[bP, b_V], writes=[bO])
                        S.group("pe", fd, reads=[bP, b_cba], writes=[bD])

                    b_selT = Buf("selT")
                    if L == 0:
                        w_qkv = W["a_w_qkv"]
                        for h in range(16):
                            for g, dil in enumerate((1, 4, 16)):
                                cb0 = g * 6144 + h * 128
                                wq, bwq = load_w(wview(w_qkv, cb0))
                                wk, bwk = load_w(wview(w_qkv, cb0 + 2048))
                                wv, bwv = load_w(wview(w_qkv, cb0 + 4096))
                                if g == 0:
                                    kblocks = [slice(128 * i, 128 * i + 128) for i in range(7, 16)]
                                    q_runs = [(slice(1024, 1536), 512, 0), (slice(1536, 2048), 512, 512)]
                                    k_runs = [(slice(896, 1408), 512, 0), (slice(1408, 1920), 512, 512),
                                              (slice(1920, 2048), 128, 1024)]
                                elif g == 1:
                                    kblocks = [sl(r + 512 * i, 128, 4) for r in range(4) for i in range(1, 4)]
                                    q_runs = [(sl(1024 + r, 256, 4), 256, r * 256) for r in range(4)]
                                    k_runs = [(sl(512 + r, 384, 4), 384, r * 384) for r in range(4)]
                                else:
                                    kblocks = [sl(r, 128, 16) for r in range(16)]
                                    q_runs = [(sl(1024 + r, 64, 16), 64, r * 64) for r in range(16)]
                                    k_runs = [(sl(r, 128, 16), 128, r * 128) for r in range(16)]
                                proj_T(wq, bwq, QT, b_QT, q_runs)
                                proj_T(wk, bwk, KT, b_KT, k_runs)
                                proj_V(wv, bwv, kblocks)
                                if g < 2:
                                    for J in range(2):
                                        Ob, bO = nb()
                                        Db, bD = nb()
                                        for j in range(2):
                                            qk, pvl = [], []
                                            for u in range(2):
                                                qb = J * 4 + j * 2 + u
                                                if g == 0:
                                                    kprev, kdiag = qb, qb + 1
                                                else:
                                                    r, i = qb // 2, qb % 2
                                                    kprev, kdiag = r * 3 + i, r * 3 + i + 1
                                                qs = slice(qb * 128, qb * 128 + 128)
                                                qk.append((u * 256, 128, slice(kprev * 128, kprev * 128 + 128), qs, None))
                                                qk.append((u * 256 + 128, 128, slice(kdiag * 128, kdiag * 128 + 128), qs, None))
                                                oc = (j * 2 + u) * 128
                                                pvl.append((kprev, u * 256, 128, oc, True, False))
                                                pvl.append((kdiag, u * 256 + 128, 128, oc, False, True))
                                            if g == 0:
                                                msk = MK01 if (J == 0 and j == 0) else MK
                                            else:
                                                msk = MK01
                                            att_bank(qk, msk, pvl, Ob, bO, Db, bD)
                                        if g == 0:
                                            dn = accn[:, J * 512:(J + 1) * 512]
                                            dd = accd[:, J * 512:(J + 1) * 512]
                                            S.op("dve", lambda e, dn=dn, Ob=Ob: e.tensor_copy(out=dn, in_=Ob[:]),
                                                 reads=[bO], writes=[b_acc])
                                            S.op("dve", lambda e, dd=dd, Db=Db: e.tensor_copy(out=dd, in_=Db[:]),
                                                 reads=[bD], writes=[b_acc])
                                        else:
                                            vn = accn[:].rearrange("p (j r) -> p r j", r=4)[:, 2 * J:2 * J + 2, :]
                                            vd = accd[:].rearrange("p (j r) -> p r j", r=4)[:, 2 * J:2 * J + 2, :]
                                            so = Ob[:].rearrange("p (a j) -> p a j", a=2)
                                            sd = Db[:].rearrange("p (a j) -> p a j", a=2)
                                            S.op("dve", lambda e, vn=vn, so=so: e.tensor_tensor(out=vn, in0=so, in1=vn, op=ALU.add),
                                                 reads=[bO, b_acc], writes=[b_acc])
                                            S.op("dve", lambda e, vd=vd, sd=sd: e.tensor_tensor(out=vd, in0=sd, in1=vd, op=ALU.add),
                                                 reads=[bD, b_acc], writes=[b_acc])
                                else:
                                    for J in range(2):
                                        Ob, bO = nb()
                                        Db, bD = nb()
                                        qk, pvl = [], []
                                        for rr in range(8):
                                            r = J * 8 + rr
                                            qk.append((rr * 64, 64, slice(r * 128, r * 128 + 128), slice(r * 64, r * 64 + 64), None))
                                            pvl.append((r, rr * 64, 64, rr * 64, True, True))
                                        att_bank(qk, MK2, pvl, Ob, bO, Db, bD)
                                        vn = accn[:].rearrange("p (j r) -> p r j", r=16)[:, 8 * J:8 * J + 8, :]
                                        vd = accd[:].rearrange("p (j r) -> p r j", r=16)[:, 8 * J:8 * J + 8, :]
                                        so = Ob[:].rearrange("p (a j) -> p a j", a=8)
                                        sd = Db[:].rearrange("p (a j) -> p a j", a=8)
                                        S.op("dve", lambda e, vn=vn, so=so: e.tensor_tensor(out=vn, in0=so, in1=vn, op=ALU.add),
                                             reads=[bO, b_acc], writes=[b_acc])
                                        S.op("dve", lambda e, vd=vd, sd=sd: e.tensor_tensor(out=vd, in0=sd, in1=vd, op=ALU.add),
                                             reads=[bD, b_acc], writes=[b_acc])
                            S.op("dve", lambda e: e.reciprocal(out=accd[:], in_=accd[:]), reads=[b_acc], writes=[b_acc])
                            S.op("dve", lambda e, h=h: e.tensor_tensor(out=mixT[:, h, :], in0=accn[:], in1=accd[:], op=ALU.mult),
                                 reads=[b_acc], writes=[b_mixT])
                    else:
                        km = A2("km", [128, 8], F32)
                        kmb = A2("kmb", [128, 8], BF16)
                        gm = A2("gm", [128, 64], F32)
                        gsel = A2("gsel", [128, 64], F32)
                        v8 = A2("v8", [128, 8], F32)
                        gbs = A2("gbs", [128, 64], F32)
                        selT = A2("selT", [32, T], BF16)
                        rec = A2("rec", [128, 256], F32)
                        b_km, b_gm, b_gsel, b_v8, b_gbs, b_rec = [Buf(n) for n in "km gm gsel v8 gbs rec".split()]
                        S.dma("sp", lambda e: e.dma_start(out=gbs[:], in_=c_gb), "const", writes=[b_gbs])
                        S.op("dve", lambda e: e.memset(selT[:], 0.0), writes=[b_selT])
                        for tz, bz_ in ((km, b_km), (kmb, b_km), (gm, b_gm), (gsel, b_gsel), (v8, b_v8), (rec, b_rec)):
                            S.op("dve", lambda e, tz=tz: e.memset(tz[:], 0.0), writes=[bz_])
                        for h in range(16):
                            wq, bwq = load_w(wview(W["b_w_q"], h * 128))
                            wk, bwk = load_w(wview(W["kv_w"], h * 128))
                            wv, bwv = load_w(wview(W["kv_w"], 2048 + h * 128))
                            proj_T(wq, bwq, QT, b_QT, [(slice(1024, 1536), 512, 0), (slice(1536, 2048), 512, 512)])
                            proj_T(wk, bwk, KT, b_KT, [(slice(i * 512, i * 512 + 512), 512, i * 512) for i in range(4)])
                            proj_V(wv, bwv, [slice(128 * i, 128 * i + 128) for i in range(16)])
                            S.op("dve", lambda e: e.tensor_reduce(out=km[:], in_=KT[:].rearrange("p (n j) -> p n j", j=256),
                                                                  axis=AX.X, op=ALU.add), reads=[b_KT], writes=[b_km])
                            S.op("dve", lambda e: e.tensor_scalar(out=kmb[:], in0=km[:], scalar1=1.0 / 256.0, scalar2=None,
                                                                  op0=ALU.mult), reads=[b_km], writes=[b_km])
                            bank, bb = nb()
                            S.group("pe", [
                                (lambda e, bank=bank, qt=qt: e.matmul(bank[:, qt * 8:(qt + 1) * 8], lhsT=QT[:, qt * 128:(qt + 1) * 128],
                                                                       rhs=kmb[:], start=True, stop=True))
                                for qt in range(8)], reads=[b_QT, b_km], writes=[bb])
                            S.op("dve", lambda e, bank=bank: e.tensor_tensor(out=gm[:], in0=bank[:, 0:64], in1=gbs[:], op=ALU.add),
                                 reads=[bb, b_gbs], writes=[b_gm])
                            for qt in range(8):
                                S.op("dve", lambda e, qt=qt: e.max(out=v8[:], in_=gm[:, qt * 8:(qt + 1) * 8]),
                                     reads=[b_gm], writes=[b_v8])
                                S.op("dve", lambda e, qt=qt: e.tensor_scalar(out=gsel[:, qt * 8:(qt + 1) * 8],
                                                                             in0=gm[:, qt * 8:(qt + 1) * 8], scalar1=v8[:, 2:3],
                                                                             scalar2=None, op0=ALU.is_ge),
                                     reads=[b_gm, b_v8], writes=[b_gsel])
                            S.op("dve", lambda e: e.tensor_scalar(out=gsel[:], in0=gsel[:], scalar1=1.0, scalar2=-NEG,
                                                                  op0=ALU.subtract, op1=ALU.mult), reads=[b_gsel], writes=[b_gsel])
                            S.op("dve", lambda e: e.tensor_tensor(out=gsel[:], in0=gsel[:], in1=gbs[:], op=ALU.add),
                                 reads=[b_gsel, b_gbs], writes=[b_gsel])
                            for half in range(2):
                                bank, bb = nb()
                                S.group("pe", [
                                    (lambda e, bank=bank, qt=half * 4 + j, j=j: e.transpose(
                                        bank[0:8, j * 128:(j + 1) * 128], gsel[:, qt * 8:(qt + 1) * 8], identF[:]))
                                    for j in range(4)], reads=[b_gsel, b_ident], writes=[bb])
                                S.op("act", lambda e, bank=bank, half=half: e.copy(out=selT[0:8, half * 512:(half + 1) * 512],
                                                                                 in_=bank[0:8, :]), reads=[bb, b_selT], writes=[b_selT])
                            for qb in range(4):
                                cur = 4 + qb
                                del avoid_banks[:]
                                Ob, bO = nb()
                                Db, bD = nb()
                                avoid_banks.extend([Ob, Db])
                                qs = slice(qb * 256, qb * 256 + 256)
                                for n in range(cur + 1):
                                    qk, pvl = [], []
                                    for kt in range(2):
                                        ti = n * 2 + kt
                                        extra = None if n == cur else (E8[:, n * 128:(n + 1) * 128], selT[:, qs])
                                        qk.append((kt * 256, 256, slice(ti * 128, ti * 128 + 128), qs, extra))
                                        pvl.append((ti, kt * 256, 256, 0, (n == 0 and kt == 0), (n == cur and kt == 1)))
                                    att_bank(qk, CM if n == cur else None, pvl, Ob, bO, Db, bD)
                                del avoid_banks[:]
                                S.op("dve", lambda e, Db=Db: e.reciprocal(out=rec[:], in_=Db[:, 0:256]), reads=[bD], writes=[b_rec])
                                S.op("dve", lambda e, Ob=Ob, h=h, qs=qs: e.tensor_tensor(out=mixT[:, h, qs], in0=Ob[:, 0:256],
                                                                                         in1=rec[:], op=ALU.mult),
                                     reads=[bO, b_rec], writes=[b_mixT])
                    S.barrier()
            S.barrier()
            top_acc = ExitStack()
            ACC = top_acc.enter_context(nc.sbuf_tensor(f"L{L}_ACC", [128, 8, D], F32))
            b_ACC = [Buf(f"ACC{i}") for i in range(8)]
            with ExitStack() as es3:
                def A3(name, shape, dt):
                    return es3.enter_context(nc.sbuf_tensor(f"L{L}_{name}", list(shape), dt))

                wo_ap = (W["a_w_o"] if L == 0 else W["b_w_o"]).rearrange("(k p) f -> p k f", p=128)
                Wo = [A3(f"Wo{i}", [128, 16, 512], BF16) for i in range(2)]
                b_Wo = [Buf("Wo0"), Buf("Wo1")]
                g_bc = A3("g_bc", [128, D], F32)
                be_bc = A3("be_bc", [128, D], F32)
                b_gbc = Buf("gbc")
                rowt = A3("rowt", [1, D], F32)
                b_rowt = Buf("rowt")
                bcast_row(g_bc, b_gbc, W["ln1_g"][L:L + 1, :], D, rowt, b_rowt)
                bcast_row(be_bc, b_gbc, W["ln1_b"][L:L + 1, :], D, rowt, b_rowt)
                rw = A3("rw", [128, 16, NE], F32)
                rb = A3("rb", [128, NE], F32)
                b_rw = Buf("rw")
                S.dma("sp", lambda e: e.dma_start(out=rw[:], in_=W["router_w"][L].rearrange("(k p) e -> p k e", p=128)), "const",
                      writes=[b_rw])
                bcast_row(rb, b_rw, W["router_b"][L:L + 1, :], NE, rowt, b_rowt)
                hT = A3("hT", [128, 16, 128], F32)
                b_hT = Buf("hT")
                st = A3("ln_st", [128, 4, 6], F32)
                mv = A3("ln_mv", [128, 2], F32)
                rstd = A3("ln_rstd", [128, 1], F32)
                b_st = Buf("lnst")
                for tt in range(8):
                    S.dma("sp", lambda e, tt=tt: e.dma_start(out=ACC[:, tt, :], in_=x_own[tt * 128:(tt + 1) * 128, :]), "xacc",
                          writes=[b_ACC[tt]])
                for n in range(4):
                    S.dma("pool", lambda e, n=n: e.dma_start(out=Wo[n % 2][:], in_=wo_ap[:, :, n * 512:(n + 1) * 512]), "Wo",
                          writes=[b_Wo[n % 2]])
                    for tt in range(8):
                        bank, bb = nb()
                        S.group("pe", [
                            (lambda e, bank=bank, h=h, n=n, tt=tt: e.matmul(bank[:], lhsT=mixT[:, h, tt * 128:(tt + 1) * 128],
                                                                           rhs=Wo[n % 2][:, h, :], start=(h == 0), stop=(h == 15)))
                            for h in range(16)], reads=[b_mixT, b_Wo[n % 2]], writes=[bb])
                        S.op("dve", lambda e, bank=bank, n=n, tt=tt: e.scalar_tensor_tensor(
                            out=ACC[:, tt, n * 512:(n + 1) * 512], in0=ACC[:, tt, n * 512:(n + 1) * 512], scalar=ALPHA, in1=bank[:],
                            op0=ALU.mult, op1=ALU.add), reads=[bb, b_ACC[tt]], writes=[b_ACC[tt]])
                for tt in range(8):
                    zs = (lambda c0, c1, tt=tt: ACC[:, tt, c0:c1])
                    bz = b_ACC[tt]
                    emit_ln(zs, bz, st, mv, rstd, b_st, g_bc, be_bc, b_gbc)
                    if dbg == f"mid{L}":
                        final_deps.append(S.dma("sp", lambda e, tt=tt: e.dma_start(
                            out=dbg_out[tt * 128:(tt + 1) * 128, :], in_=ACC[:, tt, :]), "dbg", reads=[bz]))
                    for qd in range(4):
                        bank, bb = nb()
                        S.group("pe", [
                            (lambda e, bank=bank, tt=tt, k=qd * 4 + kk, kk=kk: e.transpose(
                                bank[:, kk * 128:(kk + 1) * 128], ACC[:, tt, k * 128:(k + 1) * 128], identF[:]))
                            for kk in range(4)], reads=[bz, b_ident], writes=[bb])
                        S.op("act", lambda e, bank=bank, qd=qd: e.copy(out=hT[:, qd * 4:(qd + 1) * 4, :],
                                                                       in_=bank[:].rearrange("p (k t) -> p k t", k=4)),
                             reads=[bb], writes=[b_hT])
                    bank, bb = nb()
                    S.group("pe", [
                        (lambda e, bank=bank, k=k: e.matmul(bank[:, 0:NE], lhsT=hT[:, k, :], rhs=rw[:, k, :],
                                                            start=(k == 0), stop=(k == 15)))
                        for k in range(16)], reads=[b_hT, b_rw], writes=[bb])
                    S.op("dve", lambda e, bank=bank, tt=tt: e.tensor_tensor(out=LG[:, tt, :], in0=bank[:, 0:NE], in1=rb[:],
                                                                            op=ALU.add), reads=[bb, b_rw], writes=[b_LG])
                    S.barrier()
                S.barrier()
            top_mix.close()
            S.barrier()
            if dbg == f"mid{L}":
                top_acc.close()
                top_lg.close()
                return
            return top_acc, ACC, b_ACC

        if part != "C":
            top_acc, ACC, b_ACC = phases_AB()
        else:
            top_acc = ExitStack()
            ACC = top_acc.enter_context(nc.sbuf_tensor(f"L{L}_ACC", [128, 8, D], F32))
            b_ACC = [Buf(f"ACC{i}") for i in range(8)]
            for tt in range(8):
                S.dma("sp", lambda e, tt=tt: e.dma_start(out=ACC[:, tt, :], in_=x_own[tt * 128:(tt + 1) * 128, :]), "xacc",
                      writes=[b_ACC[tt]])
                S.dma("sp", lambda e, tt=tt: e.dma_start(out=LG[:, tt, :], in_=lgin[tt * 128:(tt + 1) * 128, :]), "lgin",
                      writes=[b_LG])
            top_mix.close()
            S.barrier()
        if part == "AB":
            for tt in range(8):
                final_deps.append(S.dma("sp", lambda e, tt=tt: e.dma_start(out=dst[tt * 128:(tt + 1) * 128, :], in_=ACC[:, tt, :]),
                                        f"oab{tt}", reads=[b_ACC[tt]]))
                final_deps.append(S.dma("sp", lambda e, tt=tt: e.dma_start(out=lgout[tt * 128:(tt + 1) * 128, :], in_=LG[:, tt, :]),
                                        f"olg{tt}", reads=[b_LG]))
            top_acc.close()
            top_lg.close()
            return
        if dbg == "gdump":
            for tt in range(8):
                final_deps.append(S.dma("sp", lambda e, tt=tt: e.dma_start(
                    out=dbg_out[tt * 128:(tt + 1) * 128, 224:256], in_=LG[:, tt, :]), f"dbgq{tt}", reads=[b_LG]))
                final_deps.append(S.dma("sp", lambda e, tt=tt: e.dma_start(
                    out=dbg_out[tt * 128:(tt + 1) * 128, 256:2048], in_=ACC[:, tt, 256:2048]), f"dbga{tt}", reads=[b_ACC[tt]]))
            S.barrier()
            import os as _os4
            if _os4.environ.get("GD_EARLY"):
                top_acc.close()
                top_lg.close()
                return
        if dbg == "lg":
            for tt in range(8):
                final_deps.append(S.dma("sp", lambda e, tt=tt: e.dma_start(
                    out=dbg_out[tt * 128:(tt + 1) * 128, 0:NE], in_=LG[:, tt, :]), f"dbg{tt}", reads=[b_LG]))
            top_acc.close()
            top_lg.close()
            return
        if dbg == "acc0":
            for tt in range(8):
                final_deps.append(S.dma("sp", lambda e, tt=tt: e.dma_start(
                    out=dbg_out[tt * 128:(tt + 1) * 128, :], in_=ACC[:, tt, :]), "dbg", reads=[b_ACC[tt]]))
            top_acc.close()
            top_lg.close()
            return
        with ExitStack() as es4:
            def A4(name, shape, dt):
                return es4.enter_context(nc.sbuf_tensor(f"L{L}_{name}", list(shape), dt))

            Hb = A4("Hb", [128, 8, D], BF16)
            b_Hb = Buf("Hb")
            cbm = A4("cbm", [128, CBM_W], BF16)
            cfm = A4("cfm", [128, CFM_W], F32)
            b_cm = Buf("cm")
            S.dma("sp", lambda e: e.dma_start(out=cbm[:], in_=c_cbm), "const", writes=[b_cm])
            S.dma("sp", lambda e: e.dma_start(out=cfm[:], in_=c_cfm), "const", writes=[b_cm])
            ones_m = cbm[:, 0:128]
            ltri = cbm[:, 128:256]
            iotaC = cfm[:, 0:256]
            G = A4("G", [128, 8, NE], F32)
            mask = A4("mask", [128, 8, NE], F32)
            maskb = A4("maskb", [128, 8, NE], BF16)
            slot = A4("slot", [128, 8, NE], F32)
            Ghl = A4("Ghl", [128, 8, NE, 2], BF16)
            slotT = A4("slotT", [32, T], BF16)
            bgT = A4("bgT", [128, 32, NE], F32)
            v8 = A4("v8m", [128, 8], F32)
            negm = A4("negm", [128, 1], F32)
            den = A4("den", [128, 1], F32)
            gtmp = A4("gtmp", [128, NE], F32)
            b_G, b_mask, b_slot, b_Ghl, b_slotT, b_bgT, b_v8, b_gtmp = [Buf(n) for n in
                                                                       "G mask slot Ghl slotT bgT v8 gtmp".split()]
            with ExitStack() as es5:
                def A5(name, shape, dt):
                    return es5.enter_context(nc.sbuf_tensor(f"L{L}_{name}", list(shape), dt))

                if dbg == "acc2":
                    for tt in range(8):
                        S.op("act", lambda e, tt=tt: e.copy(out=Hb[:, tt, :], in_=ACC[:, tt, :]), reads=[b_ACC[tt]], writes=[b_Hb])
                        S.op("dve", lambda e, tt=tt: e.tensor_scalar(out=ACC[:, tt, :], in0=ACC[:, tt, :], scalar1=ALPHA, scalar2=None,
                                                                     op0=ALU.mult), reads=[b_ACC[tt], b_Hb], writes=[b_ACC[tt]])
                        final_deps.append(S.dma("sp", lambda e, tt=tt: e.dma_start(
                            out=dbg_out[tt * 128:(tt + 1) * 128, :], in_=ACC[:, tt, :]), f"dbg{tt}", reads=[b_ACC[tt]]))
                    es5.close(); es4.close(); top_acc.close(); top_lg.close()
                    return
                bd_sb = A5("bd_sb", [128, D], F32)
                bgraw = A5("bgraw", [128, 4096], F32)
                GTs = A5("GTs", [128, 128], F32)
                b_bd, b_bgraw, b_GTs = Buf("bd"), Buf("bgraw"), Buf("GTs")
                import os as _os2
                if not _os2.environ.get("SKIP_MS"):
                    S.op("dve", lambda e: e.memset(bd_sb[:], 0.0), writes=[b_bd])
                    S.op("dve", lambda e: e.memset(bgraw[:], 0.0), writes=[b_bgraw])
                    S.op("dve", lambda e: e.memset(GTs[:], 0.0), writes=[b_GTs])
                if not _os2.environ.get("SKIP_DMA"):
                    S.dma("sp", lambda e: e.dma_start(out=bd_sb[0:32, :], in_=W["b_dn"][L]), "const", reads=[b_bd], writes=[b_bd])
                    S.dma("sp", lambda e: e.dma_start(out=bgraw[0:32, :], in_=W["b_gu"][L]), "const", reads=[b_bgraw], writes=[b_bgraw])
                import os as _os
                for half in range(0 if _os.environ.get("SKIP_BGT") else 2):
                    bank, bb = nb()
                    S.group("pe", [
                        (lambda e, bank=bank, c=half * 16 + j, j=j: e.transpose(
                            bank[:, j * 32:(j + 1) * 32], bgraw[:, c * 128:(c + 1) * 128], identF[:, 0:32]))
                        for j in range(16)], reads=[b_bgraw, b_ident], writes=[bb])
                    S.op("act", lambda e, bank=bank, half=half: e.copy(
                        out=bgT[:, half * 16:(half + 1) * 16, :], in_=bank[:].rearrange("p (c e) -> p c e", e=32)),
                        reads=[bb], writes=[b_bgT])
                if dbg == "gdump":
                    for tt in range(8):
                        final_deps.append(S.dma("sp", lambda e, tt=tt: e.dma_start(
                            out=dbg_out[tt * 128:(tt + 1) * 128, 192:224], in_=LG[:, tt, :]), f"dbgp{tt}", reads=[b_LG, b_bgT]))
                for tt in range(8):
                    S.op("act", lambda e, tt=tt: e.copy(out=Hb[:, tt, :], in_=ACC[:, tt, :]), reads=[b_ACC[tt]], writes=[b_Hb])
                    S.op("dve", lambda e, tt=tt: e.tensor_scalar(out=ACC[:, tt, :], in0=ACC[:, tt, :], scalar1=ALPHA, scalar2=None,
                                                                 op0=ALU.mult), reads=[b_ACC[tt], b_Hb], writes=[b_ACC[tt]])
                    if dbg == "acc1":
                        final_deps.append(S.dma("sp", lambda e, tt=tt: e.dma_start(
                            out=dbg_out[tt * 128:(tt + 1) * 128, :], in_=ACC[:, tt, :]), "dbg", reads=[b_ACC[tt]]))
                    lg = LG[:, tt, :]
                    S.op("dve", lambda e, lg=lg: e.max(out=v8[:], in_=lg), reads=[b_LG], writes=[b_v8])
                    if dbg == "gdump":
                        S.op("dve", lambda e, tt=tt: e.tensor_copy(out=slot[:, tt, 0:8], in_=v8[:]), reads=[b_v8], writes=[b_slot])
                        final_deps.append(S.dma("sp", lambda e, tt=tt: e.dma_start(
                            out=dbg_out[tt * 128:(tt + 1) * 128, 128:136], in_=slot[:, tt, 0:8]), f"dbgv{tt}", reads=[b_slot]))
                        final_deps.append(S.dma("sp", lambda e, tt=tt: e.dma_start(
                            out=dbg_out[tt * 128:(tt + 1) * 128, 160:192], in_=LG[:, tt, :]), f"dbgl{tt}", reads=[b_LG]))
                    S.op("dve", lambda e, lg=lg, tt=tt: e.tensor_scalar(out=mask[:, tt, :], in0=lg, scalar1=v8[:, 3:4], scalar2=None,
                                                                        op0=ALU.is_ge), reads=[b_LG, b_v8], writes=[b_mask])
                    S.op("dve", lambda e: e.tensor_scalar(out=negm[:], in0=v8[:, 0:1], scalar1=-1.0, scalar2=None, op0=ALU.mult),
                         reads=[b_v8], writes=[b_v8])
                    S.op("act", lambda e, lg=lg: e.activation(out=gtmp[:], in_=lg, func=AF.Exp, bias=negm[:, 0:1], scale=1.0),
                         reads=[b_LG, b_v8], writes=[b_gtmp])
                    S.op("dve", lambda e, tt=tt: e.tensor_tensor(out=gtmp[:], in0=gtmp[:], in1=mask[:, tt, :], op=ALU.mult),
                         reads=[b_gtmp, b_mask], writes=[b_gtmp])
                    S.op("dve", lambda e: e.tensor_reduce(out=den[:], in_=gtmp[:], axis=AX.X, op=ALU.add),
                         reads=[b_gtmp], writes=[b_v8])
                    S.op("dve", lambda e: e.reciprocal(out=den[:], in_=den[:]), reads=[b_v8], writes=[b_v8])
                    S.op("dve", lambda e, tt=tt: e.tensor_scalar(out=G[:, tt, :], in0=gtmp[:], scalar1=den[:, 0:1], scalar2=None,
                                                                 op0=ALU.mult), reads=[b_gtmp, b_v8], writes=[b_G])
                    S.op("dve", lambda e, tt=tt: e.tensor_copy(out=maskb[:, tt, :], in_=mask[:, tt, :]), reads=[b_mask],
                         writes=[b_mask])
                    S.op("dve", lambda e, tt=tt: e.tensor_copy(out=Ghl[:, tt, :, 0], in_=G[:, tt, :]), reads=[b_G], writes=[b_Ghl])
                    S.op("dve", lambda e, tt=tt: e.tensor_tensor(out=gtmp[:], in0=G[:, tt, :], in1=Ghl[:, tt, :, 0],
                                                                 op=ALU.subtract), reads=[b_G, b_Ghl], writes=[b_gtmp])
                    S.op("dve", lambda e, tt=tt: e.tensor_copy(out=Ghl[:, tt, :, 1], in_=gtmp[:]), reads=[b_gtmp], writes=[b_Ghl])
                    bank, bb = nb()
                    fns = [(lambda e, bank=bank, t2=t2: e.matmul(bank[:, 0:NE], lhsT=ones_m, rhs=maskb[:, t2, :],
                                                                  start=(t2 == 0), stop=False)) for t2 in range(tt)]
                    fns.append(lambda e, bank=bank, tt=tt: e.matmul(bank[:, 0:NE], lhsT=ltri, rhs=maskb[:, tt, :],
                                                                    start=(tt == 0), stop=True))
                    S.group("pe", fns, reads=[b_mask, b_cm], writes=[bb])
                    S.op("dve", lambda e, bank=bank, tt=tt: e.scalar_tensor_tensor(
                        out=slot[:, tt, :], in0=bank[:, 0:NE], scalar=1.0, in1=mask[:, tt, :], op0=ALU.add, op1=ALU.mult),
                        reads=[bb, b_mask], writes=[b_slot])
                    S.op("dve", lambda e, tt=tt: e.tensor_scalar(out=slot[:, tt, :], in0=slot[:, tt, :], scalar1=-1.0,
                                                                 scalar2=None, op0=ALU.add), reads=[b_slot], writes=[b_slot])
                    bank, bb = nb()
                    S.op("pe", lambda e, bank=bank, tt=tt: e.transpose(bank[0:32, 0:128], slot[:, tt, :], identF[:]),
                         reads=[b_slot, b_ident], writes=[bb])
                    S.op("act", lambda e, bank=bank, tt=tt: e.copy(out=slotT[:, tt * 128:(tt + 1) * 128], in_=bank[0:32, 0:128]),
                         reads=[bb], writes=[b_slotT])
                    bank, bb = nb()
                    S.op("pe", lambda e, bank=bank, tt=tt: e.transpose(bank[0:32, 0:128], G[:, tt, :], identF[:]),
                         reads=[b_G, b_ident], writes=[bb])
                    S.op("act", lambda e, bank=bank: e.copy(out=GTs[0:32, :], in_=bank[0:32, 0:128]), reads=[bb, b_GTs], writes=[b_GTs])
                    for n in range(4):
                        bank, bb = nb()
                        S.op("pe", lambda e, bank=bank, n=n: e.matmul(bank[:], lhsT=GTs[:], rhs=bd_sb[:, n * 512:(n + 1) * 512],
                                                                     start=True, stop=True), reads=[b_GTs, b_bd], writes=[bb])
                        S.op("dve", lambda e, bank=bank, n=n, tt=tt: e.tensor_tensor(
                            out=ACC[:, tt, n * 512:(n + 1) * 512], in0=bank[:], in1=ACC[:, tt, n * 512:(n + 1) * 512], op=ALU.add),
                            reads=[bb, b_ACC[tt]], writes=[b_ACC[tt]])
                S.barrier()
            if dbg == "gdump":
                for tt in range(8):
                    final_deps.append(S.dma("sp", lambda e, tt=tt: e.dma_start(
                        out=dbg_out[tt * 128:(tt + 1) * 128, 0:NE], in_=G[:, tt, :]), f"dbg{tt}", reads=[b_G]))
                    final_deps.append(S.dma("sp", lambda e, tt=tt: e.dma_start(
                        out=dbg_out[tt * 128:(tt + 1) * 128, NE:2 * NE], in_=slot[:, tt, :]), f"dbgs{tt}", reads=[b_slot]))
                    final_deps.append(S.dma("sp", lambda e, tt=tt: e.dma_start(
                        out=dbg_out[tt * 128:(tt + 1) * 128, 2 * NE:3 * NE], in_=mask[:, tt, :]), f"dbgm{tt}", reads=[b_mask]))
                es4.close()
                top_acc.close()
                top_lg.close()
                return
            if dbg == "acc1":
                top_acc.close()
                top_lg.close()
                return
            if dbg == "acc":
                for tt in range(8):
                    final_deps.append(S.dma("sp", lambda e, tt=tt: e.dma_start(
                        out=dbg_out[tt * 128:(tt + 1) * 128, :], in_=ACC[:, tt, :]), "dbg", reads=[b_ACC[tt]]))
                top_acc.close()
                top_lg.close()
                return
            with ExitStack() as es6:
                def A6(name, shape, dt):
                    if n_exp == 0:
                        return None
                    return es6.enter_context(nc.sbuf_tensor(f"L{L}_{name}", list(shape), dt))

                NU = 5
                un = [A6(f"un{i}", [128, 16, 256], BF16) for i in range(NU)]
                b_un = [Buf(f"un{i}") for i in range(NU)]
                un_ctr = [0]
                Sel = A6("Sel", [128, 8, CAP], BF16)
                SelT = A6("SelT", [128, 2, T], BF16)
                XeT = A6("XeT", [128, 16, CAP], BF16)
                hidT = A6("hidT", [128, 16, CAP], BF16)
                yw = A6("yw", [128, 2, D], BF16)
                wsl = A6("wsl", [128, 2], F32)
                gp = A6("gp", [128, CAP], F32)
                sg = A6("sg", [128, CAP], F32)
                tl = A6("tl", [128, CAP], F32)
                gt = A6("gt", [128, CAP], F32)
                uu = A6("uu", [128, CAP], F32)
                b_Sel, b_SelT, b_XeT, b_hidT, b_yw, b_wsl, b_gp, b_sg, b_tl, b_gt, b_uu = [Buf(n) for n in
                    "Sel SelT XeT hidT yw wsl gp sg tl gt uu".split()]
                SIG7 = float(1.0 / (1.0 + np.exp(-SW_ALPHA * SW_LIM)))

                def load_u(src_ap):
                    i = un_ctr[0] % NU
                    un_ctr[0] += 1
                    S.dma("pool", lambda e, i=i, src_ap=src_ap: e.dma_start(out=un[i][:], in_=src_ap), f"un{i}",
                          writes=[b_un[i]])
                    return un[i], b_un[i]

                for ex in range(n_exp):
                    wg_ap = W[f"w_gu{L}"][ex].rearrange("(k p) f -> p k f", p=128)
                    wd_ap = W[f"w_dn{L}"][ex].rearrange("(k p) f -> p k f", p=128)
                    for tt in range(8):
                        eng = "dve" if tt % 2 == 0 else "pool"
                        S.op(eng, lambda e, tt=tt, ex=ex: e.tensor_scalar(out=Sel[:, tt, :], in0=iotaC, scalar1=slot[:, tt, ex:ex + 1],
                                                                          scalar2=None, op0=ALU.is_equal),
                             reads=[b_slot, b_cm], writes=[b_Sel])
                    for th in range(2):
                        bank, bb = nb()
                        S.op("pe", lambda e, bank=bank, th=th, ex=ex: e.matmul(
                            bank[:], lhsT=cbm[0:32, 256 + ex * 128:256 + (ex + 1) * 128], rhs=slotT[:, th * 512:(th + 1) * 512],
                            start=True, stop=True), reads=[b_slotT, b_cm], writes=[bb])
                        for sb in range(2):
                            S.op("dve", lambda e, bank=bank, th=th, sb=sb: e.tensor_scalar(
                                out=SelT[:, sb, th * 512:(th + 1) * 512], in0=bank[:], scalar1=cfm[:, 256 + sb:257 + sb], scalar2=None,
                                op0=ALU.is_equal), reads=[bb, b_cm], writes=[b_SelT])
                    bank, bb = nb()
                    fns = []
                    for sb in range(2):
                        for tt in range(8):
                            fns.append(lambda e, bank=bank, sb=sb, tt=tt, ex=ex: e.matmul(
                                bank[:, sb * 2:sb * 2 + 2], lhsT=Sel[:, tt, sb * 128:(sb + 1) * 128], rhs=Ghl[:, tt, ex, :],
                                start=(tt == 0), stop=(tt == 7)))
                    S.group("pe", fns, reads=[b_Sel, b_Ghl], writes=[bb])
                    S.op("dve", lambda e, bank=bank: e.tensor_reduce(out=wsl[:], in_=bank[:, 0:4].rearrange("p (s t) -> p s t", t=2),
                                                                     axis=AX.X, op=ALU.add), reads=[bb], writes=[b_wsl])
                    for kp in range(8):
                        bank, bb = nb()
                        fns = []
                        for u in range(2):
                            k = kp * 2 + u
                            for tt in range(8):
                                fns.append(lambda e, bank=bank, u=u, k=k, tt=tt: e.matmul(
                                    bank[:, u * CAP:(u + 1) * CAP], lhsT=Hb[:, tt, k * 128:(k + 1) * 128], rhs=Sel[:, tt, :],
                                    start=(tt == 0), stop=(tt == 7)))
                        S.group("pe", fns, reads=[b_Hb, b_Sel], writes=[bb])
                        S.op("act", lambda e, bank=bank, kp=kp: e.copy(out=XeT[:, kp * 2:kp * 2 + 2, :],
                                                                       in_=bank[:].rearrange("p (u c) -> p u c", u=2)),
                             reads=[bb], writes=[b_XeT])
                    for j in range(8):
                        ug, bug = load_u(wg_ap[:, :, j * 256:(j + 1) * 256])
                        ul, bul = load_u(wg_ap[:, :, 2048 + j * 256:2048 + (j + 1) * 256])
                        for ff in range(2):
                            f = j * 2 + ff
                            bank, bb = nb()
                            fns = [(lambda e, bank=bank, k=k, ff=ff, ug=ug: e.matmul(
                                bank[:, 0:CAP], lhsT=ug[:, k, ff * 128:(ff + 1) * 128], rhs=XeT[:, k, :],
                                start=(k == 0), stop=(k == 15))) for k in range(16)]
                            fns += [(lambda e, bank=bank, k=k, ff=ff, ul=ul: e.matmul(
                                bank[:, CAP:2 * CAP], lhsT=ul[:, k, ff * 128:(ff + 1) * 128], rhs=XeT[:, k, :],
                                start=(k == 0), stop=(k == 15))) for k in range(16)]
                            S.group("pe", fns, reads=[bug, bul, b_XeT], writes=[bb])
                            S.op("dve", lambda e, bank=bank, f=f, ex=ex: e.tensor_scalar(
                                out=gp[:], in0=bank[:, 0:CAP], scalar1=bgT[:, f, ex:ex + 1], scalar2=None, op0=ALU.add),
                                reads=[bb, b_bgT], writes=[b_gp])
                            S.op("dve", lambda e, bank=bank, f=f, ex=ex: e.tensor_scalar(
                                out=tl[:], in0=bank[:, CAP:2 * CAP], scalar1=bgT[:, 16 + f, ex:ex + 1], scalar2=SW_LIM,
                                op0=ALU.add, op1=ALU.min), reads=[bb, b_bgT], writes=[b_tl])
                            S.op("act", lambda e: e.activation(out=sg[:], in_=gp[:], func=AF.Exp, scale=-SW_ALPHA),
                                 reads=[b_gp], writes=[b_sg])
                            S.op("dve", lambda e: e.tensor_scalar(out=sg[:], in0=sg[:], scalar1=1.0, scalar2=None, op0=ALU.add),
                                 reads=[b_sg], writes=[b_sg])
                            S.op("dve", lambda e: e.reciprocal(out=sg[:], in_=sg[:]), reads=[b_sg], writes=[b_sg])
                            S.op("pool", lambda e: e.tensor_scalar(out=gt[:], in0=gp[:], scalar1=SW_LIM, scalar2=None, op0=ALU.min),
                                 reads=[b_gp], writes=[b_gt])
                            S.op("pool", lambda e: e.tensor_scalar(out=tl[:], in0=tl[:], scalar1=-SW_LIM, scalar2=1.0,
                                                                   op0=ALU.max, op1=ALU.add), reads=[b_tl], writes=[b_tl])
                            S.op("dve", lambda e: e.scalar_tensor_tensor(out=uu[:], in0=sg[:], scalar=SIG7, in1=gt[:],
                                                                         op0=ALU.min, op1=ALU.mult),
                                 reads=[b_sg, b_gt], writes=[b_uu])
                            S.op("pool", lambda e, f=f: e.tensor_tensor(out=hidT[:, f, :], in0=tl[:], in1=uu[:], op=ALU.mult),
                                 reads=[b_tl, b_uu], writes=[b_hidT])
                    for up in range(4):
                        banks = [nb() for _ in range(2)]
                        for u2 in range(2):
                            u = up * 2 + u2
                            ud, bud = load_u(wd_ap[:, :, u * 256:(u + 1) * 256])
                            for sb in range(2):
                                bank, bb = banks[sb]
                                S.group("pe", [
                                    (lambda e, bank=bank, f=f, sb=sb, u2=u2, ud=ud: e.matmul(
                                        bank[:, u2 * 256:(u2 + 1) * 256], lhsT=hidT[:, f, sb * 128:(sb + 1) * 128], rhs=ud[:, f, :],
                                        start=(f == 0), stop=(f == 15))) for f in range(16)],
                                    reads=[b_hidT, bud], writes=[bb])
                        for sb in range(2):
                            bank, bb = banks[sb]
                            S.op("act", lambda e, bank=bank, sb=sb, up=up: e.activation(
                                out=yw[:, sb, up * 512:(up + 1) * 512], in_=bank[:], func=AF.Identity, scale=wsl[:, sb:sb + 1]),
                                reads=[bb, b_wsl], writes=[b_yw])
                    for tt in range(8):
                        for n in range(4):
                            bank, bb = nb()
                            S.group("pe", [
                                (lambda e, bank=bank, sb=sb, tt=tt, n=n: e.matmul(
                                    bank[:], lhsT=SelT[:, sb, tt * 128:(tt + 1) * 128], rhs=yw[:, sb, n * 512:(n + 1) * 512],
                                    start=(sb == 0), stop=(sb == 1))) for sb in range(2)],
                                reads=[b_SelT, b_yw], writes=[bb])
                            S.op("dve", lambda e, bank=bank, n=n, tt=tt: e.tensor_tensor(
                                out=ACC[:, tt, n * 512:(n + 1) * 512], in0=bank[:], in1=ACC[:, tt, n * 512:(n + 1) * 512], op=ALU.add),
                                reads=[bb, b_ACC[tt]], writes=[b_ACC[tt]])
                S.barrier()
            with ExitStack() as es7:
                def A7(name, shape, dt):
                    return es7.enter_context(nc.sbuf_tensor(f"L{L}_{name}", list(shape), dt))

                g_bc = A7("g2_bc", [128, D], F32)
                be_bc = A7("be2_bc", [128, D], F32)
                b_gbc = Buf("gbc2")
                rowt = A7("rowt2", [1, D], F32)
                b_rowt = Buf("rowt2")
                bcast_row(g_bc, b_gbc, W["ln2_g"][L:L + 1, :], D, rowt, b_rowt)
                bcast_row(be_bc, b_gbc, W["ln2_b"][L:L + 1, :], D, rowt, b_rowt)
                st = A7("ln2_st", [128, 4, 6], F32)
                mv = A7("ln2_mv", [128, 2], F32)
                rstd = A7("ln2_rstd", [128, 1], F32)
                b_st = Buf("ln2st")
                if dbg == "ln2in":
                    for tt in range(8):
                        final_deps.append(S.dma("sp", lambda e, tt=tt: e.dma_start(
                            out=dbg_out[tt * 128:(tt + 1) * 128, :], in_=ACC[:, tt, :]), f"dbg{tt}", reads=[b_ACC[tt]]))
                    es7.close(); es4.close(); top_acc.close(); top_lg.close()
                    return
                for tt in range(8):
                    emit_ln((lambda c0, c1, tt=tt: ACC[:, tt, c0:c1]), b_ACC[tt], st, mv, rstd, b_st, g_bc, be_bc, b_gbc)
                    p = S.dma("sp", lambda e, tt=tt: e.dma_start(out=dst[tt * 128:(tt + 1) * 128, :], in_=ACC[:, tt, :]), "dst",
                              reads=[b_ACC[tt]])
                    if dst_is_out:
                        final_deps.append(p)
                    else:
                        b_dsts.readers.append(p)
                S.barrier()
        top_acc.close()
        top_lg.close()
        S.barrier()

    b_dsts = Buf("dsts")
    if len(layers) == 1:
        run_layer(layers[0], xo, xp, out, True)
    else:
        raise NotImplementedError("fused build added later")
    S.emit(final_deps)
    print("[kernel] instruction counts", S.n_inst, flush=True)
    return nc


_PROGS = {}


def _get_prog(layers):
    if layers not in _PROGS:
        _PROGS[layers] = build_program(layers)
    return _PROGS[layers]


def _core_maps(layer, src, inputs, n_exp=NE, moe=True, part=None, lg=None):
    maps = []
    f32 = lambda a: np.ascontiguousarray(a, dtype=np.float32)
    shared = {
        "router_w": f32(inputs["router_w"]), "router_b": f32(inputs["router_b"]),
        "ln1_g": f32(inputs["ln1_g"]), "ln1_b": f32(inputs["ln1_b"]),
        "ln2_g": f32(inputs["ln2_g"]), "ln2_b": f32(inputs["ln2_b"]),
    }
    if moe:
        ne = max(n_exp, 1)
        shared[f"w_gu{layer}"] = f32(inputs["moe_w_gate_up"][layer][:ne])
        shared[f"w_dn{layer}"] = f32(inputs["moe_w_down"][layer][:ne])
        shared["moe_b_gate_up"] = f32(inputs["moe_b_gate_up"])
        shared["moe_b_down"] = f32(inputs["moe_b_down"])
    if part == "C":
        pass
    elif layer == 0:
        shared["a_w_qkv"] = f32(inputs["a_w_qkv"][0])
        shared["a_w_o"] = f32(inputs["a_w_o"][0])
    else:
        shared["kv_w"] = f32(inputs["kv_w"])
        shared["b_w_q"] = f32(inputs["b_w_q"][0])
        shared["b_w_o"] = f32(inputs["b_w_o"][0])
    consts = [host_consts(0), host_consts(1)]
    for c in range(8):
        b, hf = c // 2, c % 2
        m = dict(shared)
        m["xo"] = f32(src[b, hf * T:(hf + 1) * T])
        m["xp"] = f32(src[b, 0:T])
        if lg is not None:
            m["lgin"] = f32(lg[c])
        m.update(consts[hf])
        maps.append(m)
    return maps


def _gather(res):
    o = np.zeros((4, 2048, D), np.float32)
    for c in range(8):
        b, hf = c // 2, c % 2
        o[b, hf * T:(hf + 1) * T] = res.results[c]["out"]
    return o


def _get_prog_part(layers, part):
    key = (layers, part)
    if key not in _PROGS:
        _PROGS[key] = build_program(layers, part=part)
    return _PROGS[key]


def kernel(**inputs):
    x = np.asarray(inputs["x"], dtype=np.float32)
    ids = list(range(8))
    r0 = run_bass_kernel_spmd(_get_prog((0,)), _core_maps(0, x, inputs), core_ids=ids)
    h1 = _gather(r0)
    ra = run_bass_kernel_spmd(_get_prog_part((1,), "AB"), _core_maps(1, h1, inputs, moe=False, part="AB"), core_ids=ids)
    hm = _gather(ra)
    lg = [ra.results[c]["lgout"] for c in range(8)]
    rc_ = run_bass_kernel_spmd(_get_prog_part((1,), "C"), _core_maps(1, hm, inputs, part="C", lg=lg), core_ids=ids)
    return _gather(rc_)
```

```python
import numpy as np
import ml_dtypes
from contextlib import ExitStack
import concourse.bass as bass
import concourse.mybir as mybir
from concourse.bass_utils import run_bass_kernel_spmd

F32 = mybir.dt.float32
BF16 = mybir.dt.bfloat16
AF = mybir.ActivationFunctionType
ALU = mybir.AluOpType
AX = mybir.AxisListType

SEM_EPOCH = 30000
D = 2048
T = 1024
NE = 32
CAP = 256
ALPHA = 4.0 ** 0.25
EPS = 1e-5
SCALE = 128.0 ** -0.5
SW_ALPHA = 1.702
SW_LIM = 7.0
NEG = -30000.0


def sl(start, count, step):
    return slice(start, start + (count - 1) * step + 1, step)


class Buf:
    __slots__ = ("name", "last_w", "readers", "excl")

    def __init__(self, name="", excl=False):
        self.name = name
        self.last_w = None
        self.readers = []
        self.excl = excl


class Sched:
    ENGS = ("pe", "act", "dve", "pool", "sp")

    def __init__(self, nc):
        self.nc = nc
        self.ops = {e: [] for e in self.ENGS}
        self.cnt = {e: 0 for e in self.ENGS}
        self.sem = {e: nc.alloc_semaphore(name=f"s_{e}_0") for e in self.ENGS}
        self.epoch = {e: 0 for e in self.ENGS}
        self.seen = {e: {} for e in self.ENGS}
        self.dma_sems = {}
        self.free_dma = []
        self.n_dma_sem = 0
        self.all_dma = []
        self.old_sems = []
        self.n_inst = {e: 0 for e in self.ENGS}

    def _wait(self, eng, dep):
        sem, val = dep
        key = id(sem)
        if self.seen[eng].get(key, 0) >= val:
            return
        self.seen[eng][key] = val
        self.ops[eng].append(("wait", sem, val))

    def _deps(self, eng, reads, writes):
        deps = []
        for b in reads:
            if b.last_w is not None:
                deps.append(b.last_w)
            if b.excl:
                deps.extend(b.readers)
        for b in writes:
            if b.last_w is not None:
                deps.append(b.last_w)
            deps.extend(b.readers)
        for d in deps:
            if eng == "pe" and d[0] is self.sem["pe"]:
                continue
            self._wait(eng, d)

    def _record(self, produced, reads, writes):
        for b in writes:
            b.last_w = produced
            b.readers = []
        for b in reads:
            if b.last_w is produced:
                continue
            b.readers.append(produced)

    def _next(self, eng):
        if self.cnt[eng] >= SEM_EPOCH:
            self.old_sems.append((self.sem[eng], self.cnt[eng]))
            self.epoch[eng] += 1
            self.sem[eng] = self.nc.alloc_semaphore(name=f"s_{eng}_{self.epoch[eng]}")
            self.cnt[eng] = 0
        self.cnt[eng] += 1
        return (self.sem[eng], self.cnt[eng])

    def op(self, eng, fn, reads=(), writes=()):
        self._deps(eng, reads, writes)
        produced = self._next(eng)
        self.ops[eng].append(("op", fn, produced[0], 1))
        self._record(produced, reads, writes)
        self.n_inst[eng] += 1

    def group(self, eng, fns, reads=(), writes=()):
        self._deps(eng, reads, writes)
        for fn in fns[:-1]:
            self.ops[eng].append(("op", fn, None, 0))
        produced = self._next(eng)
        self.ops[eng].append(("op", fns[-1], produced[0], 1))
        self._record(produced, reads, writes)
        self.n_inst[eng] += len(fns)

    def dma(self, eng, fn, semname, reads=(), writes=()):
        self._deps(eng, reads, writes)
        if writes:
            semname = f"{semname}_{writes[0].name}_{id(writes[0])}"
        ent = self.dma_sems.get(semname)
        if ent is None and self.free_dma:
            ent = self.free_dma.pop()
            self.dma_sems[semname] = ent
        if ent is None or ent[1] + 16 > SEM_EPOCH:
            if ent is not None:
                self.old_sems.append((ent[0], ent[1]))
            idx = 0 if ent is None else ent[2] + 1
            self.n_dma_sem += 1
            ent = [self.nc.alloc_semaphore(name=f"d{self.n_dma_sem}_{idx}"), 0, idx]
            self.dma_sems[semname] = ent
        ent[1] += 16
        produced = (ent[0], ent[1])
        self.ops[eng].append(("op", fn, ent[0], 16))
        self._record(produced, reads, writes)
        self.n_inst[eng] += 1
        return produced

    def barrier(self):
        deps = []
        for e in self.ENGS:
            if self.cnt[e] > 0:
                deps.append((self.sem[e], self.cnt[e]))
        for ent in self.dma_sems.values():
            deps.append((ent[0], ent[1]))
        deps.extend(self.old_sems)
        for e in self.ENGS:
            for d in deps:
                if d[0] is self.sem[e]:
                    continue
                self._wait(e, d)
        for ent in self.dma_sems.values():
            if not any(ent is x for x in self.all_dma):
                self.all_dma.append(ent)
            self.free_dma.append(ent)
        self.dma_sems = {}

    def emit(self, final_deps):
        for d in final_deps:
            self._wait("sp", d)
        ops = self.ops

        def run(e, lst):
            for it in lst:
                if it[0] == "wait":
                    e.wait_ge(it[1], it[2])
                else:
                    ins = it[1](e)
                    if it[2] is not None:
                        ins.then_inc(it[2], it[3])

        sems = ([h for h in self.sem.values()] + [h for (h, _) in self.old_sems] + [ent[0] for ent in self.dma_sems.values()]
                + [ent[0] for ent in self.all_dma] + [ent[0] for ent in self.free_dma])
        uniq = []
        for h in sems:
            if not any(h is u for u in uniq):
                uniq.append(h)
        self.nc.all_engine_barrier()
        for h in uniq:
            self.nc.gpsimd.sem_clear(h)
        self.nc.all_engine_barrier()
        with self.nc.Block() as block:
            @block.tensor
            def _(e):
                run(e, ops["pe"])

            @block.scalar
            def _(e):
                run(e, ops["act"])

            @block.vector
            def _(e):
                run(e, ops["dve"])

            @block.gpsimd
            def _(e):
                run(e, ops["pool"])

            @block.sync
            def _(e):
                run(e, ops["sp"])
        self.nc.all_engine_barrier()
        for h in uniq:
            self.nc.gpsimd.sem_clear(h)
        self.nc.all_engine_barrier()


CBA_W = 32 + 128 + 512 * 4 + 1024
CBM_W = 128 + 128 + 4096
CFM_W = 256 + 2


def host_consts(hf):
    pv = 1.0 if hf == 1 else 0.0
    k = np.arange(128)[:, None]
    q = np.arange(128)[None, :]
    A = (k <= q).astype(np.float32)
    B = (k >= q).astype(np.float32)
    cba = np.zeros((128, CBA_W), np.float32)
    o = 0
    perm = np.zeros((32, 32), np.float32)
    for m in range(32):
        perm[(m + 16) % 32, m] = 1.0
    cba[0:32, o:o + 32] = perm
    o += 32
    cba[:, o:o + 128] = 1.0
    o += 128
    cba[:, o:o + 512] = np.concatenate([B * pv, A, B, A], axis=1)
    o += 512
    cba[:, o:o + 512] = np.concatenate([B, A, B, A], axis=1)
    o += 512
    q64 = np.arange(64)[None, :]
    m2 = ((k <= 64 + q64) & ((k >= 64) | (pv > 0))).astype(np.float32)
    cba[:, o:o + 512] = np.tile(m2, (1, 8))
    o += 512
    q256 = np.arange(256)[None, :]
    cba[:, o:o + 512] = np.concatenate([(k <= q256), (128 + k <= q256)], axis=1).astype(np.float32)
    o += 512
    for n in range(8):
        cba[n, o + n * 128:o + (n + 1) * 128] = 1.0
    o += 1024
    assert o == CBA_W
    cbm = np.zeros((128, CBM_W), np.float32)
    cbm[:, 0:128] = 1.0
    cbm[:, 128:256] = (k < q).astype(np.float32)
    for e in range(32):
        cbm[e, 256 + e * 128:256 + (e + 1) * 128] = 1.0
    cfm = np.zeros((128, CFM_W), np.float32)
    cfm[:, 0:256] = np.arange(256, dtype=np.float32)[None, :]
    cfm[:, 256] = np.arange(128)
    cfm[:, 257] = np.arange(128) + 128
    gb = np.zeros((128, 8, 8), np.float32)
    for qt in range(8):
        cur = 4 + qt // 2
        for n in range(8):
            if n >= cur or (n < 4 and pv == 0.0):
                gb[:, qt, n] = NEG
    pos = np.arange(2048, dtype=np.float32)
    if hf == 0:
        pos = np.maximum(pos - 1024.0, 0.0).astype(np.float32)
    inv = (np.float32(500000.0) ** (-(np.arange(0, 32, 2, dtype=np.float32)) / np.float32(32.0))).astype(np.float32)
    ang = (pos[None, :] * inv[:, None]).astype(np.float32)
    c = np.cos(ang).astype(np.float32)
    s = np.sin(ang).astype(np.float32)
    rc = np.concatenate([c, c], axis=0)
    rs = np.concatenate([-s, s], axis=0)
    return dict(cba=cba.astype(ml_dtypes.bfloat16), cbm=cbm.astype(ml_dtypes.bfloat16), cfm=cfm,
                gbias=gb.reshape(128, 64), rc=np.ascontiguousarray(rc), rs=np.ascontiguousarray(rs))


def build_program(layers, n_exp=NE, dbg=None, part=None):
    nc = bass.Bass("TRN2", target_bir_lowering=False)
    S = Sched(nc)

    def din(name, shape, dt=F32):
        return nc.dram_tensor(name, list(shape), dt, kind="ExternalInput").ap()

    xo = din("xo", [T, D])
    xp = din("xp", [T, D])
    W = {}
    if 0 in layers and part != "C":
        W["a_w_qkv"] = din("a_w_qkv", [D, 18432])
        W["a_w_o"] = din("a_w_o", [D, D])
    if 1 in layers and part != "C":
        W["kv_w"] = din("kv_w", [D, 4096])
        W["b_w_q"] = din("b_w_q", [D, D])
        W["b_w_o"] = din("b_w_o", [D, D])
    lgin = din("lgin", [T, NE]) if part == "C" else None
    W["router_w"] = din("router_w", [2, D, NE])
    W["router_b"] = din("router_b", [2, NE])
    import os as _os3
    if part != "AB" and (_os3.environ.get("FORCE_MOE") or not ((dbg or "").startswith("mid") or dbg in ("acc0", "lg"))):
        for L_ in layers:
            W[f"w_gu{L_}"] = din(f"w_gu{L_}", [max(n_exp, 1), D, 4096])
            W[f"w_dn{L_}"] = din(f"w_dn{L_}", [max(n_exp, 1), D, D])
        W["b_gu"] = din("moe_b_gate_up", [2, NE, 4096])
        W["b_dn"] = din("moe_b_down", [2, NE, D])
    for nm in ("ln1_g", "ln1_b", "ln2_g", "ln2_b"):
        W[nm] = din(nm, [2, D])
    c_cba = din("cba", [128, CBA_W], BF16)
    c_cbm = din("cbm", [128, CBM_W], BF16)
    c_cfm = din("cfm", [128, CFM_W])
    c_gb = din("gbias", [128, 64])
    c_rc = din("rc", [32, 2048])
    c_rs = din("rs", [32, 2048])
    out = nc.dram_tensor("out", [T, D], F32, kind="ExternalOutput").ap()
    lgout = nc.dram_tensor("lgout", [T, NE], F32, kind="ExternalOutput").ap() if part == "AB" else None
    dbg_out = None
    if dbg is not None:
        dbg_out = nc.dram_tensor("dbg", [T, D], F32, kind="ExternalOutput").ap()
    hmid = None
    if len(layers) == 2:
        hmid = nc.dram_tensor("hmid", [T, D], F32, kind="Internal").ap()

    identF = nc.alloc_sbuf_tensor("identF", [128, 128], F32)
    b_ident = Buf("ident")
    S.op("pool", lambda e: e.memset(identF[:], 0.0), writes=[b_ident])
    S.op("pool", lambda e: e.affine_select(out=identF[:], in_=identF[:], pattern=[[-1, 128]],
                                           compare_op=ALU.not_equal, fill=1.0, base=0, channel_multiplier=1),
         reads=[b_ident], writes=[b_ident])
    psb = [nc.alloc_psum_tensor(f"ps{i}", [128, 512], F32) for i in range(8)]
    b_ps = [Buf(f"ps{i}", excl=True) for i in range(8)]
    bank_ctr = [0]

    avoid_banks = []

    def nb():
        while True:
            i = bank_ctr[0] % 8
            bank_ctr[0] += 1
            if not any(psb[i] is a for a in avoid_banks):
                return psb[i], b_ps[i]

    final_deps = []

    ones_row = nc.alloc_sbuf_tensor("ones_row", [1, 128], F32)
    b_onesrow = Buf("ones_row")
    S.op("dve", lambda e: e.memset(ones_row[:], 1.0), writes=[b_onesrow])

    def bcast_row(dst_t, b_dst, src_row_ap, n, row_t, b_row):
        S.dma("sp", lambda e: e.dma_start(out=row_t[0:1, 0:n], in_=src_row_ap), "brow", writes=[b_row])
        for c0 in range(0, n, 512):
            c1 = min(n, c0 + 512)
            bank, bb = nb()
            S.op("pe", lambda e, bank=bank, c0=c0, c1=c1: e.matmul(bank[:, 0:c1 - c0], lhsT=ones_row[0:1, :], rhs=row_t[0:1, c0:c1],
                                                                    start=True, stop=True), reads=[b_row, b_onesrow], writes=[bb])
            S.op("act", lambda e, bank=bank, c0=c0, c1=c1: e.copy(out=dst_t[:, c0:c1], in_=bank[:, 0:c1 - c0]),
                 reads=[bb], writes=[b_dst])

    def emit_ln(zs, bz, st, mv, rstd, b_st, g_bc, be_bc, b_gbc):
        zf = zs(0, D)
        for c in range(4):
            S.op("dve", lambda e, c=c: e.bn_stats(out=st[:, c, :], in_=zs(c * 512, (c + 1) * 512)), reads=[bz], writes=[b_st])
        S.op("dve", lambda e: e.bn_aggr(out=mv[:], in_=st[:].rearrange("p c s -> p (c s)")), reads=[b_st], writes=[b_st])
        S.op("dve", lambda e: e.tensor_scalar(out=rstd[:], in0=mv[:, 1:2], scalar1=EPS, scalar2=None, op0=ALU.add),
             reads=[b_st], writes=[b_st])
        S.op("act", lambda e: e.activation(out=rstd[:], in_=rstd[:], func=AF.Ln), reads=[b_st], writes=[b_st])
        S.op("act", lambda e: e.activation(out=rstd[:], in_=rstd[:], func=AF.Exp, scale=-0.5), reads=[b_st], writes=[b_st])
        S.op("dve", lambda e: e.tensor_scalar(out=zf, in0=zf, scalar1=mv[:, 0:1], scalar2=rstd[:, 0:1],
                                              op0=ALU.subtract, op1=ALU.mult), reads=[bz, b_st], writes=[bz])
        S.op("pool", lambda e: e.tensor_tensor(out=zf, in0=zf, in1=g_bc[:], op=ALU.mult), reads=[bz, b_gbc], writes=[bz])
        S.op("dve", lambda e: e.tensor_tensor(out=zf, in0=zf, in1=be_bc[:], op=ALU.add), reads=[bz, b_gbc], writes=[bz])

    def run_layer(L, x_own, x_prev, dst, dst_is_out):
        top_lg = ExitStack()
        top_mix = ExitStack()
        LG = top_lg.enter_context(nc.sbuf_tensor(f"L{L}_LG", [128, 8, NE], F32))
        b_LG = Buf("LG")
        _pad = top_lg.enter_context(nc.sbuf_tensor(f"L{L}_pad", [128, 256], F32))
        mixT = top_mix.enter_context(nc.sbuf_tensor(f"L{L}_mixT", [128, 16, T], BF16, side="right"))
        b_mixT = Buf("mixT")
        def phases_AB():
            with ExitStack() as es:
                def A(name, shape, dt):
                    return es.enter_context(nc.sbuf_tensor(f"L{L}_{name}", list(shape), dt))

                XT = A("XT", [128, 16, 2048], BF16)
                b_XT = Buf("XT")
                cba = A("cba", [128, CBA_W], BF16)
                b_cba = Buf("cba")
                rc = A("rc", [32, 2048], F32)
                rs = A("rs", [32, 2048], F32)
                b_rope = Buf("rope")
                S.dma("sp", lambda e: e.dma_start(out=cba[:], in_=c_cba), "const", writes=[b_cba])
                S.dma("sp", lambda e: e.dma_start(out=rc[:], in_=c_rc), "const", writes=[b_rope])
                S.dma("sp", lambda e: e.dma_start(out=rs[:], in_=c_rs), "const", writes=[b_rope])
                perm32 = cba[0:32, 0:32]
                ones_b = cba[:, 32:160]
                MK01 = cba[:, 160:672]
                MK = cba[:, 672:1184]
                MK2 = cba[:, 1184:1696]
                CM = cba[:, 1696:2208]
                E8 = cba[0:32, 2208:3232]


                with ExitStack() as es1:
                    xs = [es1.enter_context(nc.sbuf_tensor(f"L{L}_xs{i}", [128, D], F32)) for i in range(2)]
                    b_xs = [Buf("xs0"), Buf("xs1")]
                    for t in range(16):
                        src = x_prev[t * 128:(t + 1) * 128, :] if t < 8 else x_own[(t - 8) * 128:(t - 7) * 128, :]
                        xi = xs[t % 2]
                        bx = b_xs[t % 2]
                        S.dma("sp", lambda e, xi=xi, src=src: e.dma_start(out=xi[:], in_=src), "xs", writes=[bx])
                        for qd in range(4):
                            bank, bb = nb()
                            S.group("pe", [
                                (lambda e, bank=bank, xi=xi, k=qd * 4 + kk, kk=kk:
                                 e.transpose(bank[:, kk * 128:(kk + 1) * 128], xi[:, k * 128:(k + 1) * 128], identF[:]))
                                for kk in range(4)], reads=[bx, b_ident], writes=[bb])
                            dst_ap = XT[:, qd * 4:(qd + 1) * 4, t * 128:(t + 1) * 128]
                            src_ap = bank[:].rearrange("p (k t) -> p k t", k=4)
                            if (t * 4 + qd) % 2 == 0:
                                S.op("act", lambda e, d=dst_ap, s=src_ap: e.copy(out=d, in_=s), reads=[bb], writes=[b_XT])
                            else:
                                S.op("dve", lambda e, d=dst_ap, s=src_ap: e.tensor_copy(out=d, in_=s), reads=[bb], writes=[b_XT])
                    S.barrier()

                with ExitStack() as es2:
                    def A2(name, shape, dt):
                        return es2.enter_context(nc.sbuf_tensor(f"L{L}_{name}", list(shape), dt))

                    wu = [A2(f"wu{i}", [128, 16, 128], BF16) for i in range(6)]
                    b_wu = [Buf(f"wu{i}") for i in range(6)]
                    QT = A2("QT", [128, T], BF16)
                    KT = A2("KT", [128, 2048], BF16)
                    V = A2("V", [128, 16, 128], BF16)
                    b_QT, b_KT, b_V = Buf("QT"), Buf("KT"), Buf("V")
                    accn = A2("accn", [128, T], F32)
                    accd = A2("accd", [128, T], F32)
                    b_acc = Buf("acc")
                    PT = [A2(f"PT{i}", [128, 512], BF16) for i in range(2)]
                    b_PT = [Buf("PT0"), Buf("PT1")]
                    t1 = A2("rt1", [32, 512], F32)
                    t2 = A2("rt2", [32, 512], F32)
                    b_t1, b_t2 = Buf("t1"), Buf("t2")
                    wu_ctr = [0]
                    pt_ctr = [0]
                    for tz, bz_ in ((QT, b_QT), (KT, b_KT), (V, b_V), (accn, b_acc), (accd, b_acc), (PT[0], b_PT[0]), (PT[1], b_PT[1]),
                                    (t1, b_t1), (t2, b_t2)):
                        S.op("dve", lambda e, tz=tz: e.memset(tz[:], 0.0), writes=[bz_])
                    for i in range(6):
                        S.op("dve", lambda e, i=i: e.memset(wu[i][:], 0.0), writes=[b_wu[i]])

                    def load_w(src_ap):
                        i = wu_ctr[0] % 6
                        wu_ctr[0] += 1
                        S.dma("pool", lambda e, i=i, src_ap=src_ap: e.dma_start(out=wu[i][:], in_=src_ap), f"wu{i}",
                              writes=[b_wu[i]])
                        return wu[i], b_wu[i]

                    def wview(w_ap, c0):
                        return w_ap.rearrange("(k p) f -> p k f", p=128)[:, :, c0:c0 + 128]

                    def proj_T(w, bw, dstT, b_dst, runs):
                        for (ps, N, o0) in runs:
                            bank, bb = nb()
                            S.group("pe", [
                                (lambda e, bank=bank, k=k, ps=ps, N=N: e.matmul(bank[:, 0:N], lhsT=w[:, k, :], rhs=XT[:, k, ps],
                                                                              start=(k == 0), stop=(k == 15)))
                                for k in range(16)], reads=[bw, b_XT], writes=[bb])
                            S.op("act", lambda e, bank=bank, N=N, o0=o0: e.copy(out=dstT[:, o0:o0 + N], in_=bank[:, 0:N]),
                                 reads=[bb], writes=[b_dst])
                            bank2, bb2 = nb()
                            S.op("pe", lambda e, bank2=bank2, N=N, o0=o0: e.matmul(bank2[0:32, 0:N], lhsT=perm32,
                                                                                  rhs=dstT[0:32, o0:o0 + N], start=True, stop=True),
                                 reads=[b_dst, b_cba], writes=[bb2])
                            S.op("dve", lambda e, bank=bank, N=N, ps=ps: e.tensor_tensor(out=t1[:, 0:N], in0=bank[0:32, 0:N],
                                                                                         in1=rc[:, ps], op=ALU.mult),
                                 reads=[bb, b_rope], writes=[b_t1])
                            S.op("dve", lambda e, bank2=bank2, N=N, ps=ps: e.tensor_tensor(out=t2[:, 0:N], in0=bank2[0:32, 0:N],
                                                                                           in1=rs[:, ps], op=ALU.mult),
                                 reads=[bb2, b_rope], writes=[b_t2])
                            S.op("dve", lambda e, N=N, o0=o0: e.tensor_tensor(out=dstT[0:32, o0:o0 + N], in0=t1[:, 0:N],
                                                                              in1=t2[:, 0:N], op=ALU.add),
                                 reads=[b_t1, b_t2], writes=[b_dst])

                    def proj_V(w, bw, blocks):
                        for b0 in range(0, len(blocks), 4):
                            bank, bb = nb()
                            fns = []
                            chunk = blocks[b0:b0 + 4]
                            for j, ps in enumerate(chunk):
                                for k in range(16):
                                    fns.append(lambda e, bank=bank, j=j, ps=ps, k=k: e.matmul(
                                        bank[:, j * 128:(j + 1) * 128], lhsT=XT[:, k, ps], rhs=w[:, k, :],
                                        start=(k == 0), stop=(k == 15)))
                            S.group("pe", fns, reads=[bw, b_XT], writes=[bb])
                            n = len(chunk)
                            S.op("act", lambda e, bank=bank, b0=b0, n=n: e.copy(
                                out=V[:, b0:b0 + n, :], in_=bank[:, 0:n * 128].rearrange("p (j c) -> p j c", c=128)),
                                reads=[bb], writes=[b_V])

                    def att_bank(qk_list, mask_ap, pv_list, Ob, bO, Db, bD):
                        bank, bb = nb()
                        fns = []
                        rd = [b_KT, b_QT]
                        for (c0, N, kcols, qcols, extra) in qk_list:
                            if extra is None:
                                fns.append(lambda e, bank=bank, c0=c0, N=N, kcols=kcols, qcols=qcols: e.matmul(
                                    bank[:, c0:c0 + N], lhsT=KT[:, kcols], rhs=QT[:, qcols], start=True, stop=True))
                            else:
                                el, er = extra
                                fns.append(lambda e, bank=bank, c0=c0, N=N, kcols=kcols, qcols=qcols: e.matmul(
                                    bank[:, c0:c0 + N], lhsT=KT[:, kcols], rhs=QT[:, qcols], start=True, stop=False))
                                fns.append(lambda e, bank=bank, c0=c0, N=N, el=el, er=er: e.matmul(
                                    bank[:, c0:c0 + N], lhsT=el, rhs=er, start=False, stop=True))
                        S.group("pe", fns, reads=rd + [b_cba, b_selT], writes=[bb])
                        pi = pt_ctr[0] % 2
                        pt_ctr[0] += 1
                        P, bP = PT[pi], b_PT[pi]
                        S.op("act", lambda e, bank=bank, P=P: e.activation(out=P[:], in_=bank[:], func=AF.Exp, scale=SCALE),
                             reads=[bb], writes=[bP])
                        if mask_ap is not None:
                            S.op("dve", lambda e, P=P, m=mask_ap: e.tensor_tensor(out=P[:], in0=P[:], in1=m, op=ALU.mult),
                                 reads=[bP, b_cba], writes=[bP])
                        fo, fd = [], []
                        for (vi, pc, N, oc, st, sp) in pv_list:
                            fo.append(lambda e, vi=vi, pc=pc, N=N, oc=oc, st=st, sp=sp, P=P: e.matmul(
                                Ob[:, oc:oc + N], lhsT=V[:, vi, :], rhs=P[:, pc:pc + N], start=st, stop=sp))
                            fd.append(lambda e, pc=pc, N=N, oc=oc, st=st, sp=sp, P=P: e.matmul(
                                Db[:, oc:oc + N], lhsT=ones_b, rhs=P[:, pc:pc + N], start=st, stop=sp))
                        S.group("pe", fo, reads=[bP, b_V], writes=[bO])
                        S.group("pe", fd, reads=[bP, b_cba], writes=[bD])

                    b_selT = Buf("selT")
                    if L == 0:
                        w_qkv = W["a_w_qkv"]
                        for h in range(16):
                            for g, dil in enumerate((1, 4, 16)):
                                cb0 = g * 6144 + h * 128
                                wq, bwq = load_w(wview(w_qkv, cb0))
                                wk, bwk = load_w(wview(w_qkv, cb0 + 2048))
                                wv, bwv = load_w(wview(w_qkv, cb0 + 4096))
                                if g == 0:
                                    kblocks = [slice(128 * i, 128 * i + 128) for i in range(7, 16)]
                                    q_runs = [(slice(1024, 1536), 512, 0), (slice(1536, 2048), 512, 512)]
                                    k_runs = [(slice(896, 1408), 512, 0), (slice(1408, 1920), 512, 512),
                                              (slice(1920, 2048), 128, 1024)]
                                elif g == 1:
                                    kblocks = [sl(r + 512 * i, 128, 4) for r in range(4) for i in range(1, 4)]
                                    q_runs = [(sl(1024 + r, 256, 4), 256, r * 256) for r in range(4)]
                                    k_runs = [(sl(512 + r, 384, 4), 384, r * 384) for r in range(4)]
                                else:
                                    kblocks = [sl(r, 128, 16) for r in range(16)]
                                    q_runs = [(sl(1024 + r, 64, 16), 64, r * 64) for r in range(16)]
                                    k_runs = [(sl(r, 128, 16), 128, r * 128) for r in range(16)]
                                proj_T(wq, bwq, QT, b_QT, q_runs)
                                proj_T(wk, bwk, KT, b_KT, k_runs)
                                proj_V(wv, bwv, kblocks)
                                if g < 2:
                                    for J in range(2):
                                        Ob, bO = nb()
                                        Db, bD = nb()
                                        for j in range(2):
                                            qk, pvl = [], []
                                            for u in range(2):
                                                qb = J * 4 + j * 2 + u
                                                if g == 0:
                                                    kprev, kdiag = qb, qb + 1
                                                else:
                                                    r, i = qb // 2, qb % 2
                                                    kprev, kdiag = r * 3 + i, r * 3 + i + 1
                                                qs = slice(qb * 128, qb * 128 + 128)
                                                qk.append((u * 256, 128, slice(kprev * 128, kprev * 128 + 128), qs, None))
                                                qk.append((u * 256 + 128, 128, slice(kdiag * 128, kdiag * 128 + 128), qs, None))
                                                oc = (j * 2 + u) * 128
                                                pvl.append((kprev, u * 256, 128, oc, True, False))
                                                pvl.append((kdiag, u * 256 + 128, 128, oc, False, True))
                                            if g == 0:
                                                msk = MK01 if (J == 0 and j == 0) else MK
                                            else:
                                                msk = MK01
                                            att_bank(qk, msk, pvl, Ob, bO, Db, bD)
                                        if g == 0:
                                            dn = accn[:, J * 512:(J + 1) * 512]
                                            dd = accd[:, J * 512:(J + 1) * 512]
                                            S.op("dve", lambda e, dn=dn, Ob=Ob: e.tensor_copy(out=dn, in_=Ob[:]),
                                                 reads=[bO], writes=[b_acc])
                                            S.op("dve", lambda e, dd=dd, Db=Db: e.tensor_copy(out=dd, in_=Db[:]),
                                                 reads=[bD], writes=[b_acc])
                                        else:
                                            vn = accn[:].rearrange("p (j r) -> p r j", r=4)[:, 2 * J:2 * J + 2, :]
                                            vd = accd[:].rearrange("p (j r) -> p r j", r=4)[:, 2 * J:2 * J + 2, :]
                                            so = Ob[:].rearrange("p (a j) -> p a j", a=2)
                                            sd = Db[:].rearrange("p (a j) -> p a j", a=2)
                                            S.op("dve", lambda e, vn=vn, so=so: e.tensor_tensor(out=vn, in0=so, in1=vn, op=ALU.add),
                                                 reads=[bO, b_acc], writes=[b_acc])
                                            S.op("dve", lambda e, vd=vd, sd=sd: e.tensor_tensor(out=vd, in0=sd, in1=vd, op=ALU.add),
                                                 reads=[bD, b_acc], writes=[b_acc])
                                else:
                                    for J in range(2):
                                        Ob, bO = nb()
                                        Db, bD = nb()
                                        qk, pvl = [], []
                                        for rr in range(8):
                                            r = J * 8 + rr
                                            qk.append((rr * 64, 64, slice(r * 128, r * 128 + 128), slice(r * 64, r * 64 + 64), None))
                                            pvl.append((r, rr * 64, 64, rr * 64, True, True))
                                        att_bank(qk, MK2, pvl, Ob, bO, Db, bD)
                                        vn = accn[:].rearrange("p (j r) -> p r j", r=16)[:, 8 * J:8 * J + 8, :]
                                        vd = accd[:].rearrange("p (j r) -> p r j", r=16)[:, 8 * J:8 * J + 8, :]
                                        so = Ob[:].rearrange("p (a j) -> p a j", a=8)
                                        sd = Db[:].rearrange("p (a j) -> p a j", a=8)
                                        S.op("dve", lambda e, vn=vn, so=so: e.tensor_tensor(out=vn, in0=so, in1=vn, op=ALU.add),
                                             reads=[bO, b_acc], writes=[b_acc])
                                        S.op("dve", lambda e, vd=vd, sd=sd: e.tensor_tensor(out=vd, in0=sd, in1=vd, op=ALU.add),
                                             reads=[bD, b_acc], writes=[b_acc])
                            S.op("dve", lambda e: e.reciprocal(out=accd[:], in_=accd[:]), reads=[b_acc], writes=[b_acc])
                            S.op("dve", lambda e, h=h: e.tensor_tensor(out=mixT[:, h, :], in0=accn[:], in1=accd[:], op=ALU.mult),
                                 reads=[b_acc], writes=[b_mixT])
                    else:
                        km = A2("km", [128, 8], F32)
                        kmb = A2("kmb", [128, 8], BF16)
                        gm = A2("gm", [128, 64], F32)
                        gsel = A2("gsel", [128, 64], F32)
                        v8 = A2("v8", [128, 8], F32)
                        gbs = A2("gbs", [128, 64], F32)
                        selT = A2("selT", [32, T], BF16)
                        rec = A2("rec", [128, 256], F32)
                        b_km, b_gm, b_gsel, b_v8, b_gbs, b_rec = [Buf(n) for n in "km gm gsel v8 gbs rec".split()]
                        S.dma("sp", lambda e: e.dma_start(out=gbs[:], in_=c_gb), "const", writes=[b_gbs])
                        S.op("dve", lambda e: e.memset(selT[:], 0.0), writes=[b_selT])
                        for tz, bz_ in ((km, b_km), (kmb, b_km), (gm, b_gm), (gsel, b_gsel), (v8, b_v8), (rec, b_rec)):
                            S.op("dve", lambda e, tz=tz: e.memset(tz[:], 0.0), writes=[bz_])
                        for h in range(16):
                            wq, bwq = load_w(wview(W["b_w_q"], h * 128))
                            wk, bwk = load_w(wview(W["kv_w"], h * 128))
                            wv, bwv = load_w(wview(W["kv_w"], 2048 + h * 128))
                            proj_T(wq, bwq, QT, b_QT, [(slice(1024, 1536), 512, 0), (slice(1536, 2048), 512, 512)])
                            proj_T(wk, bwk, KT, b_KT, [(slice(i * 512, i * 512 + 512), 512, i * 512) for i in range(4)])
                            proj_V(wv, bwv, [slice(128 * i, 128 * i + 128) for i in range(16)])
                            S.op("dve", lambda e: e.tensor_reduce(out=km[:], in_=KT[:].rearrange("p (n j) -> p n j", j=256),
                                                                  axis=AX.X, op=ALU.add), reads=[b_KT], writes=[b_km])
                            S.op("dve", lambda e: e.tensor_scalar(out=kmb[:], in0=km[:], scalar1=1.0 / 256.0, scalar2=None,
                                                                  op0=ALU.mult), reads=[b_km], writes=[b_km])
                            bank, bb = nb()
                            S.group("pe", [
                                (lambda e, bank=bank, qt=qt: e.matmul(bank[:, qt * 8:(qt + 1) * 8], lhsT=QT[:, qt * 128:(qt + 1) * 128],
                                                                       rhs=kmb[:], start=True, stop=True))
                                for qt in range(8)], reads=[b_QT, b_km], writes=[bb])
                            S.op("dve", lambda e, bank=bank: e.tensor_tensor(out=gm[:], in0=bank[:, 0:64], in1=gbs[:], op=ALU.add),
                                 reads=[bb, b_gbs], writes=[b_gm])
                            for qt in range(8):
                                S.op("dve", lambda e, qt=qt: e.max(out=v8[:], in_=gm[:, qt * 8:(qt + 1) * 8]),
                                     reads=[b_gm], writes=[b_v8])
                                S.op("dve", lambda e, qt=qt: e.tensor_scalar(out=gsel[:, qt * 8:(qt + 1) * 8],
                                                                             in0=gm[:, qt * 8:(qt + 1) * 8], scalar1=v8[:, 2:3],
                                                                             scalar2=None, op0=ALU.is_ge),
                                     reads=[b_gm, b_v8], writes=[b_gsel])
                            S.op("dve", lambda e: e.tensor_scalar(out=gsel[:], in0=gsel[:], scalar1=1.0, scalar2=-NEG,
                                                                  op0=ALU.subtract, op1=ALU.mult), reads=[b_gsel], writes=[b_gsel])
                            S.op("dve", lambda e: e.tensor_tensor(out=gsel[:], in0=gsel[:], in1=gbs[:], op=ALU.add),
                                 reads=[b_gsel, b_gbs], writes=[b_gsel])
                            for half in range(2):
                                bank, bb = nb()
                                S.group("pe", [
                                    (lambda e, bank=bank, qt=half * 4 + j, j=j: e.transpose(
                                        bank[0:8, j * 128:(j + 1) * 128], gsel[:, qt * 8:(qt + 1) * 8], identF[:]))
                                    for j in range(4)], reads=[b_gsel, b_ident], writes=[bb])
                                S.op("act", lambda e, bank=bank, half=half: e.copy(out=selT[0:8, half * 512:(half + 1) * 512],
                                                                                 in_=bank[0:8, :]), reads=[bb, b_selT], writes=[b_selT])
                            for qb in range(4):
                                cur = 4 + qb
                                del avoid_banks[:]
                                Ob, bO = nb()
                                Db, bD = nb()
                                avoid_banks.extend([Ob, Db])
                                qs = slice(qb * 256, qb * 256 + 256)
                                for n in range(cur + 1):
                                    qk, pvl = [], []
                                    for kt in range(2):
                                        ti = n * 2 + kt
                                        extra = None if n == cur else (E8[:, n * 128:(n + 1) * 128], selT[:, qs])
                                        qk.append((kt * 256, 256, slice(ti * 128, ti * 128 + 128), qs, extra))
                                        pvl.append((ti, kt * 256, 256, 0, (n == 0 and kt == 0), (n == cur and kt == 1)))
                                    att_bank(qk, CM if n == cur else None, pvl, Ob, bO, Db, bD)
                                del avoid_banks[:]
                                S.op("dve", lambda e, Db=Db: e.reciprocal(out=rec[:], in_=Db[:, 0:256]), reads=[bD], writes=[b_rec])
                                S.op("dve", lambda e, Ob=Ob, h=h, qs=qs: e.tensor_tensor(out=mixT[:, h, qs], in0=Ob[:, 0:256],
                                                                                         in1=rec[:], op=ALU.mult),
                                     reads=[bO, b_rec], writes=[b_mixT])
                    S.barrier()
            S.barrier()
            top_acc = ExitStack()
            ACC = top_acc.enter_context(nc.sbuf_tensor(f"L{L}_ACC", [128, 8, D], F32))
            b_ACC = [Buf(f"ACC{i}") for i in range(8)]
            with ExitStack() as es3:
                def A3(name, shape, dt):
                    return es3.enter_context(nc.sbuf_tensor(f"L{L}_{name}", list(shape), dt))

                wo_ap = (W["a_w_o"] if L == 0 else W["b_w_o"]).rearrange("(k p) f -> p k f", p=128)
                Wo = [A3(f"Wo{i}", [128, 16, 512], BF16) for i in range(2)]
                b_Wo = [Buf("Wo0"), Buf("Wo1")]
                g_bc = A3("g_bc", [128, D], F32)
                be_bc = A3("be_bc", [128, D], F32)
                b_gbc = Buf("gbc")
                rowt = A3("rowt", [1, D], F32)
                b_rowt = Buf("rowt")
                bcast_row(g_bc, b_gbc, W["ln1_g"][L:L + 1, :], D, rowt, b_rowt)
                bcast_row(be_bc, b_gbc, W["ln1_b"][L:L + 1, :], D, rowt, b_rowt)
                rw = A3("rw", [128, 16, NE], F32)
                rb = A3("rb", [128, NE], F32)
                b_rw = Buf("rw")
                S.dma("sp", lambda e: e.dma_start(out=rw[:], in_=W["router_w"][L].rearrange("(k p) e -> p k e", p=128)), "const",
                      writes=[b_rw])
                bcast_row(rb, b_rw, W["router_b"][L:L + 1, :], NE, rowt, b_rowt)
                hT = A3("hT", [128, 16, 128], F32)
                b_hT = Buf("hT")
                st = A3("ln_st", [128, 4, 6], F32)
                mv = A3("ln_mv", [128, 2], F32)
                rstd = A3("ln_rstd", [128, 1], F32)
                b_st = Buf("lnst")
                for tt in range(8):
                    S.dma("sp", lambda e, tt=tt: e.dma_start(out=ACC[:, tt, :], in_=x_own[tt * 128:(tt + 1) * 128, :]), "xacc",
                          writes=[b_ACC[tt]])
                for n in range(4):
                    S.dma("pool", lambda e, n=n: e.dma_start(out=Wo[n % 2][:], in_=wo_ap[:, :, n * 512:(n + 1) * 512]), "Wo",
                          writes=[b_Wo[n % 2]])
                    for tt in range(8):
                        bank, bb = nb()
                        S.group("pe", [
                            (lambda e, bank=bank, h=h, n=n, tt=tt: e.matmul(bank[:], lhsT=mixT[:, h, tt * 128:(tt + 1) * 128],
                                                                           rhs=Wo[n % 2][:, h, :], start=(h == 0), stop=(h == 15)))
                            for h in range(16)], reads=[b_mixT, b_Wo[n % 2]], writes=[bb])
                        S.op("dve", lambda e, bank=bank, n=n, tt=tt: e.scalar_tensor_tensor(
                            out=ACC[:, tt, n * 512:(n + 1) * 512], in0=ACC[:, tt, n * 512:(n + 1) * 512], scalar=ALPHA, in1=bank[:],
                            op0=ALU.mult, op1=ALU.add), reads=[bb, b_ACC[tt]], writes=[b_ACC[tt]])
                for tt in range(8):
                    zs = (lambda c0, c1, tt=tt: ACC[:, tt, c0:c1])
                    bz = b_ACC[tt]
                    emit_ln(zs, bz, st, mv, rstd, b_st, g_bc, be_bc, b_gbc)
                    if dbg == f"mid{L}":
                        final_deps.append(S.dma("sp", lambda e, tt=tt: e.dma_start(
                            out=dbg_out[tt * 128:(tt + 1) * 128, :], in_=ACC[:, tt, :]), "dbg", reads=[bz]))
                    for qd in range(4):
                        bank, bb = nb()
                        S.group("pe", [
                            (lambda e, bank=bank, tt=tt, k=qd * 4 + kk, kk=kk: e.transpose(
                                bank[:, kk * 128:(kk + 1) * 128], ACC[:, tt, k * 128:(k + 1) * 128], identF[:]))
                            for kk in range(4)], reads=[bz, b_ident], writes=[bb])
                        S.op("act", lambda e, bank=bank, qd=qd: e.copy(out=hT[:, qd * 4:(qd + 1) * 4, :],
                                                                       in_=bank[:].rearrange("p (k t) -> p k t", k=4)),
                             reads=[bb], writes=[b_hT])
                    bank, bb = nb()
                    S.group("pe", [
                        (lambda e, bank=bank, k=k: e.matmul(bank[:, 0:NE], lhsT=hT[:, k, :], rhs=rw[:, k, :],
                                                            start=(k == 0), stop=(k == 15)))
                        for k in range(16)], reads=[b_hT, b_rw], writes=[bb])
                    S.op("dve", lambda e, bank=bank, tt=tt: e.tensor_tensor(out=LG[:, tt, :], in0=bank[:, 0:NE], in1=rb[:],
                                                                            op=ALU.add), reads=[bb, b_rw], writes=[b_LG])
                    S.barrier()
                S.barrier()
            top_mix.close()
            S.barrier()
            if dbg == f"mid{L}":
                top_acc.close()
                top_lg.close()
                return
            return top_acc, ACC, b_ACC

        if part != "C":
            top_acc, ACC, b_ACC = phases_AB()
        else:
            top_acc = ExitStack()
            ACC = top_acc.enter_context(nc.sbuf_tensor(f"L{L}_ACC", [128, 8, D], F32))
            b_ACC = [Buf(f"ACC{i}") for i in range(8)]
            for tt in range(8):
                S.dma("sp", lambda e, tt=tt: e.dma_start(out=ACC[:, tt, :], in_=x_own[tt * 128:(tt + 1) * 128, :]), "xacc",
                      writes=[b_ACC[tt]])
                S.dma("sp", lambda e, tt=tt: e.dma_start(out=LG[:, tt, :], in_=lgin[tt * 128:(tt + 1) * 128, :]), "lgin",
                      writes=[b_LG])
            top_mix.close()
            S.barrier()
        if part == "AB":
            for tt in range(8):
                final_deps.append(S.dma("sp", lambda e, tt=tt: e.dma_start(out=dst[tt * 128:(tt + 1) * 128, :], in_=ACC[:, tt, :]),
                                        f"oab{tt}", reads=[b_ACC[tt]]))
                final_deps.append(S.dma("sp", lambda e, tt=tt: e.dma_start(out=lgout[tt * 128:(tt + 1) * 128, :], in_=LG[:, tt, :]),
                                        f"olg{tt}", reads=[b_LG]))
            top_acc.close()
            top_lg.close()
            return
        if dbg == "gdump":
            for tt in range(8):
                final_deps.append(S.dma("sp", lambda e, tt=tt: e.dma_start(
                    out=dbg_out[tt * 128:(tt + 1) * 128, 224:256], in_=LG[:, tt, :]), f"dbgq{tt}", reads=[b_LG]))
                final_deps.append(S.dma("sp", lambda e, tt=tt: e.dma_start(
                    out=dbg_out[tt * 128:(tt + 1) * 128, 256:2048], in_=ACC[:, tt, 256:2048]), f"dbga{tt}", reads=[b_ACC[tt]]))
            S.barrier()
            import os as _os4
            if _os4.environ.get("GD_EARLY"):
                top_acc.close()
                top_lg.close()
                return
        if dbg == "lg":
            for tt in range(8):
                final_deps.append(S.dma("sp", lambda e, tt=tt: e.dma_start(
                    out=dbg_out[tt * 128:(tt + 1) * 128, 0:NE], in_=LG[:, tt, :]), f"dbg{tt}", reads=[b_LG]))
            top_acc.close()
            top_lg.close()
            return
        if dbg == "acc0":
            for tt in range(8):
                final_deps.append(S.dma("sp", lambda e, tt=tt: e.dma_start(
                    out=dbg_out[tt * 128:(tt + 1) * 128, :], in_=ACC[:, tt, :]), "dbg", reads=[b_ACC[tt]]))
            top_acc.close()
            top_lg.close()
            return
        with ExitStack() as es4:
            def A4(name, shape, dt):
                return es4.enter_context(nc.sbuf_tensor(f"L{L}_{name}", list(shape), dt))

            Hb = A4("Hb", [128, 8, D], BF16)
            b_Hb = Buf("Hb")
            cbm = A4("cbm", [128, CBM_W], BF16)
            cfm = A4("cfm", [128, CFM_W], F32)
            b_cm = Buf("cm")
            S.dma("sp", lambda e: e.dma_start(out=cbm[:], in_=c_cbm), "const", writes=[b_cm])
            S.dma("sp", lambda e: e.dma_start(out=cfm[:], in_=c_cfm), "const", writes=[b_cm])
            ones_m = cbm[:, 0:128]
            ltri = cbm[:, 128:256]
            iotaC = cfm[:, 0:256]
            G = A4("G", [128, 8, NE], F32)
            mask = A4("mask", [128, 8, NE], F32)
            maskb = A4("maskb", [128, 8, NE], BF16)
            slot = A4("slot", [128, 8, NE], F32)
            Ghl = A4("Ghl", [128, 8, NE, 2], BF16)
            slotT = A4("slotT", [32, T], BF16)
            bgT = A4("bgT", [128, 32, NE], F32)
            v8 = A4("v8m", [128, 8], F32)
            negm = A4("negm", [128, 1], F32)
            den = A4("den", [128, 1], F32)
            gtmp = A4("gtmp", [128, NE], F32)
            b_G, b_mask, b_slot, b_Ghl, b_slotT, b_bgT, b_v8, b_gtmp = [Buf(n) for n in
                                                                       "G mask slot Ghl slotT bgT v8 gtmp".split()]
            with ExitStack() as es5:
                def A5(name, shape, dt):
                    return es5.enter_context(nc.sbuf_tensor(f"L{L}_{name}", list(shape), dt))

                if dbg == "acc2":
                    for tt in range(8):
                        S.op("act", lambda e, tt=tt: e.copy(out=Hb[:, tt, :], in_=ACC[:, tt, :]), reads=[b_ACC[tt]], writes=[b_Hb])
                        S.op("dve", lambda e, tt=tt: e.tensor_scalar(out=ACC[:, tt, :], in0=ACC[:, tt, :], scalar1=ALPHA, scalar2=None,
                                                                     op0=ALU.mult), reads=[b_ACC[tt], b_Hb], writes=[b_ACC[tt]])
                        final_deps.append(S.dma("sp", lambda e, tt=tt: e.dma_start(
                            out=dbg_out[tt * 128:(tt + 1) * 128, :], in_=ACC[:, tt, :]), f"dbg{tt}", reads=[b_ACC[tt]]))
                    es5.close(); es4.close(); top_acc.close(); top_lg.close()
                    return
                bd_sb = A5("bd_sb", [128, D], F32)
                bgraw = A5("bgraw", [128, 4096], F32)
                GTs = A5("GTs", [128, 128], F32)
                b_bd, b_bgraw, b_GTs = Buf("bd"), Buf("bgraw"), Buf("GTs")
                import os as _os2
                if not _os2.environ.get("SKIP_MS"):
                    S.op("dve", lambda e: e.memset(bd_sb[:], 0.0), writes=[b_bd])
                    S.op("dve", lambda e: e.memset(bgraw[:], 0.0), writes=[b_bgraw])
                    S.op("dve", lambda e: e.memset(GTs[:], 0.0), writes=[b_GTs])
                if not _os2.environ.get("SKIP_DMA"):
                    S.dma("sp", lambda e: e.dma_start(out=bd_sb[0:32, :], in_=W["b_dn"][L]), "const", reads=[b_bd], writes=[b_bd])
                    S.dma("sp", lambda e: e.dma_start(out=bgraw[0:32, :], in_=W["b_gu"][L]), "const", reads=[b_bgraw], writes=[b_bgraw])
                import os as _os
                for half in range(0 if _os.environ.get("SKIP_BGT") else 2):
                    bank, bb = nb()
                    S.group("pe", [
                        (lambda e, bank=bank, c=half * 16 + j, j=j: e.transpose(
                            bank[:, j * 32:(j + 1) * 32], bgraw[:, c * 128:(c + 1) * 128], identF[:, 0:32]))
                        for j in range(16)], reads=[b_bgraw, b_ident], writes=[bb])
                    S.op("act", lambda e, bank=bank, half=half: e.copy(
                        out=bgT[:, half * 16:(half + 1) * 16, :], in_=bank[:].rearrange("p (c e) -> p c e", e=32)),
                        reads=[bb], writes=[b_bgT])
                if dbg == "gdump":
                    for tt in range(8):
                        final_deps.append(S.dma("sp", lambda e, tt=tt: e.dma_start(
                            out=dbg_out[tt * 128:(tt + 1) * 128, 192:224], in_=LG[:, tt, :]), f"dbgp{tt}", reads=[b_LG, b_bgT]))
                for tt in range(8):
                    S.op("act", lambda e, tt=tt: e.copy(out=Hb[:, tt, :], in_=ACC[:, tt, :]), reads=[b_ACC[tt]], writes=[b_Hb])
                    S.op("dve", lambda e, tt=tt: e.tensor_scalar(out=ACC[:, tt, :], in0=ACC[:, tt, :], scalar1=ALPHA, scalar2=None,
                                                                 op0=ALU.mult), reads=[b_ACC[tt], b_Hb], writes=[b_ACC[tt]])
                    if dbg == "acc1":
                        final_deps.append(S.dma("sp", lambda e, tt=tt: e.dma_start(
                            out=dbg_out[tt * 128:(tt + 1) * 128, :], in_=ACC[:, tt, :]), "dbg", reads=[b_ACC[tt]]))
                    lg = LG[:, tt, :]
                    S.op("dve", lambda e, lg=lg: e.max(out=v8[:], in_=lg), reads=[b_LG], writes=[b_v8])
                    if dbg == "gdump":
                        S.op("dve", lambda e, tt=tt: e.tensor_copy(out=slot[:, tt, 0:8], in_=v8[:]), reads=[b_v8], writes=[b_slot])
                        final_deps.append(S.dma("sp", lambda e, tt=tt: e.dma_start(
                            out=dbg_out[tt * 128:(tt + 1) * 128, 128:136], in_=slot[:, tt, 0:8]), f"dbgv{tt}", reads=[b_slot]))
                        final_deps.append(S.dma("sp", lambda e, tt=tt: e.dma_start(
                            out=dbg_out[tt * 128:(tt + 1) * 128, 160:192], in_=LG[:, tt, :]), f"dbgl{tt}", reads=[b_LG]))
                    S.op("dve", lambda e, lg=lg, tt=tt: e.tensor_scalar(out=mask[:, tt, :], in0=lg, scalar1=v8[:, 3:4], scalar2=None,
                                                                        op0=ALU.is_ge), reads=[b_LG, b_v8], writes=[b_mask])
                    S.op("dve", lambda e: e.tensor_scalar(out=negm[:], in0=v8[:, 0:1], scalar1=-1.0, scalar2=None, op0=ALU.mult),
                         reads=[b_v8], writes=[b_v8])
                    S.op("act", lambda e, lg=lg: e.activation(out=gtmp[:], in_=lg, func=AF.Exp, bias=negm[:, 0:1], scale=1.0),
                         reads=[b_LG, b_v8], writes=[b_gtmp])
                    S.op("dve", lambda e, tt=tt: e.tensor_tensor(out=gtmp[:], in0=gtmp[:], in1=mask[:, tt, :], op=ALU.mult),
                         reads=[b_gtmp, b_mask], writes=[b_gtmp])
                    S.op("dve", lambda e: e.tensor_reduce(out=den[:], in_=gtmp[:], axis=AX.X, op=ALU.add),
                         reads=[b_gtmp], writes=[b_v8])
                    S.op("dve", lambda e: e.reciprocal(out=den[:], in_=den[:]), reads=[b_v8], writes=[b_v8])
                    S.op("dve", lambda e, tt=tt: e.tensor_scalar(out=G[:, tt, :], in0=gtmp[:], scalar1=den[:, 0:1], scalar2=None,
                                                                 op0=ALU.mult), reads=[b_gtmp, b_v8], writes=[b_G])
                    S.op("dve", lambda e, tt=tt: e.tensor_copy(out=maskb[:, tt, :], in_=mask[:, tt, :]), reads=[b_mask],
                         writes=[b_mask])
                    S.op("dve", lambda e, tt=tt: e.tensor_copy(out=Ghl[:, tt, :, 0], in_=G[:, tt, :]), reads=[b_G], writes=[b_Ghl])
                    S.op("dve", lambda e, tt=tt: e.tensor_tensor(out=gtmp[:], in0=G[:, tt, :], in1=Ghl[:, tt, :, 0],
                                                                 op=ALU.subtract), reads=[b_G, b_Ghl], writes=[b_gtmp])
                    S.op("dve", lambda e, tt=tt: e.tensor_copy(out=Ghl[:, tt, :, 1], in_=gtmp[:]), reads=[b_gtmp], writes=[b_Ghl])
                    bank, bb = nb()
                    fns = [(lambda e, bank=bank, t2=t2: e.matmul(bank[:, 0:NE], lhsT=ones_m, rhs=maskb[:, t2, :],
                                                                  start=(t2 == 0), stop=False)) for t2 in range(tt)]
                    fns.append(lambda e, bank=bank, tt=tt: e.matmul(bank[:, 0:NE], lhsT=ltri, rhs=maskb[:, tt, :],
                                                                    start=(tt == 0), stop=True))
                    S.group("pe", fns, reads=[b_mask, b_cm], writes=[bb])
                    S.op("dve", lambda e, bank=bank, tt=tt: e.scalar_tensor_tensor(
                        out=slot[:, tt, :], in0=bank[:, 0:NE], scalar=1.0, in1=mask[:, tt, :], op0=ALU.add, op1=ALU.mult),
                        reads=[bb, b_mask], writes=[b_slot])
                    S.op("dve", lambda e, tt=tt: e.tensor_scalar(out=slot[:, tt, :], in0=slot[:, tt, :], scalar1=-1.0,
                                                                 scalar2=None, op0=ALU.add), reads=[b_slot], writes=[b_slot])
                    bank, bb = nb()
                    S.op("pe", lambda e, bank=bank, tt=tt: e.transpose(bank[0:32, 0:128], slot[:, tt, :], identF[:]),
                         reads=[b_slot, b_ident], writes=[bb])
                    S.op("act", lambda e, bank=bank, tt=tt: e.copy(out=slotT[:, tt * 128:(tt + 1) * 128], in_=bank[0:32, 0:128]),
                         reads=[bb], writes=[b_slotT])
                    bank, bb = nb()
                    S.op("pe", lambda e, bank=bank, tt=tt: e.transpose(bank[0:32, 0:128], G[:, tt, :], identF[:]),
                         reads=[b_G, b_ident], writes=[bb])
                    S.op("act", lambda e, bank=bank: e.copy(out=GTs[0:32, :], in_=bank[0:32, 0:128]), reads=[bb, b_GTs], writes=[b_GTs])
                    for n in range(4):
                        bank, bb = nb()
                        S.op("pe", lambda e, bank=bank, n=n: e.matmul(bank[:], lhsT=GTs[:], rhs=bd_sb[:, n * 512:(n + 1) * 512],
                                                                     start=True, stop=True), reads=[b_GTs, b_bd], writes=[bb])
                        S.op("dve", lambda e, bank=bank, n=n, tt=tt: e.tensor_tensor(
                            out=ACC[:, tt, n * 512:(n + 1) * 512], in0=bank[:], in1=ACC[:, tt, n * 512:(n + 1) * 512], op=ALU.add),
                            reads=[bb, b_ACC[tt]], writes=[b_ACC[tt]])
                S.barrier()
            if dbg == "gdump":
                for tt in range(8):
                    final_deps.append(S.dma("sp", lambda e, tt=tt: e.dma_start(
                        out=dbg_out[tt * 128:(tt + 1) * 128, 0:NE], in_=G[:, tt, :]), f"dbg{tt}", reads=[b_G]))
                    final_deps.append(S.dma("sp", lambda e, tt=tt: e.dma_start(
                        out=dbg_out[tt * 128:(tt + 1) * 128, NE:2 * NE], in_=slot[:, tt, :]), f"dbgs{tt}", reads=[b_slot]))
                    final_deps.append(S.dma("sp", lambda e, tt=tt: e.dma_start(
                        out=dbg_out[tt * 128:(tt + 1) * 128, 2 * NE:3 * NE], in_=mask[:, tt, :]), f"dbgm{tt}", reads=[b_mask]))
                es4.close()
                top_acc.close()
                top_lg.close()
                return
            if dbg == "acc1":
                top_acc.close()
                top_lg.close()
                return
            if dbg == "acc":
                for tt in range(8):
                    final_deps.append(S.dma("sp", lambda e, tt=tt: e.dma_start(
                        out=dbg_out[tt * 128:(tt + 1) * 128, :], in_=ACC[:, tt, :]), "dbg", reads=[b_ACC[tt]]))
                top_acc.close()
                top_lg.close()
                return
            with ExitStack() as es6:
                def A6(name, shape, dt):
                    if n_exp == 0:
                        return None
                    return es6.enter_context(nc.sbuf_tensor(f"L{L}_{name}", list(shape), dt))

                NU = 5
                un = [A6(f"un{i}", [128, 16, 256], BF16) for i in range(NU)]
                b_un = [Buf(f"un{i}") for i in range(NU)]
                un_ctr = [0]
                Sel = A6("Sel", [128, 8, CAP], BF16)
                SelT = A6("SelT", [128, 2, T], BF16)
                XeT = A6("XeT", [128, 16, CAP], BF16)
                hidT = A6("hidT", [128, 16, CAP], BF16)
                yw = A6("yw", [128, 2, D], BF16)
                wsl = A6("wsl", [128, 2], F32)
                gp = A6("gp", [128, CAP], F32)
                sg = A6("sg", [128, CAP], F32)
                tl = A6("tl", [128, CAP], F32)
                gt = A6("gt", [128, CAP], F32)
                uu = A6("uu", [128, CAP], F32)
                b_Sel, b_SelT, b_XeT, b_hidT, b_yw, b_wsl, b_gp, b_sg, b_tl, b_gt, b_uu = [Buf(n) for n in
                    "Sel SelT XeT hidT yw wsl gp sg tl gt uu".split()]
                SIG7 = float(1.0 / (1.0 + np.exp(-SW_ALPHA * SW_LIM)))

                def load_u(src_ap):
                    i = un_ctr[0] % NU
                    un_ctr[0] += 1
                    S.dma("pool", lambda e, i=i, src_ap=src_ap: e.dma_start(out=un[i][:], in_=src_ap), f"un{i}",
                          writes=[b_un[i]])
                    return un[i], b_un[i]

                for ex in range(n_exp):
                    wg_ap = W[f"w_gu{L}"][ex].rearrange("(k p) f -> p k f", p=128)
                    wd_ap = W[f"w_dn{L}"][ex].rearrange("(k p) f -> p k f", p=128)
                    for tt in range(8):
                        eng = "dve"
                        S.op(eng, lambda e, tt=tt, ex=ex: e.tensor_scalar(out=Sel[:, tt, :], in0=iotaC, scalar1=slot[:, tt, ex:ex + 1],
                                                                          scalar2=None, op0=ALU.is_equal),
                             reads=[b_slot, b_cm], writes=[b_Sel])
                    for th in range(2):
                        bank, bb = nb()
                        S.op("pe", lambda e, bank=bank, th=th, ex=ex: e.matmul(
                            bank[:], lhsT=cbm[0:32, 256 + ex * 128:256 + (ex + 1) * 128], rhs=slotT[:, th * 512:(th + 1) * 512],
                            start=True, stop=True), reads=[b_slotT, b_cm], writes=[bb])
                        for sb in range(2):
                            S.op("dve", lambda e, bank=bank, th=th, sb=sb: e.tensor_scalar(
                                out=SelT[:, sb, th * 512:(th + 1) * 512], in0=bank[:], scalar1=cfm[:, 256 + sb:257 + sb], scalar2=None,
                                op0=ALU.is_equal), reads=[bb, b_cm], writes=[b_SelT])
                    bank, bb = nb()
                    fns = []
                    for sb in range(2):
                        for tt in range(8):
                            fns.append(lambda e, bank=bank, sb=sb, tt=tt, ex=ex: e.matmul(
                                bank[:, sb * 2:sb * 2 + 2], lhsT=Sel[:, tt, sb * 128:(sb + 1) * 128], rhs=Ghl[:, tt, ex, :],
                                start=(tt == 0), stop=(tt == 7)))
                    S.group("pe", fns, reads=[b_Sel, b_Ghl], writes=[bb])
                    S.op("dve", lambda e, bank=bank: e.tensor_reduce(out=wsl[:], in_=bank[:, 0:4].rearrange("p (s t) -> p s t", t=2),
                                                                     axis=AX.X, op=ALU.add), reads=[bb], writes=[b_wsl])
                    for kp in range(8):
                        bank, bb = nb()
                        fns = []
                        for u in range(2):
                            k = kp * 2 + u
                            for tt in range(8):
                                fns.append(lambda e, bank=bank, u=u, k=k, tt=tt: e.matmul(
                                    bank[:, u * CAP:(u + 1) * CAP], lhsT=Hb[:, tt, k * 128:(k + 1) * 128], rhs=Sel[:, tt, :],
                                    start=(tt == 0), stop=(tt == 7)))
                        S.group("pe", fns, reads=[b_Hb, b_Sel], writes=[bb])
                        S.op("act", lambda e, bank=bank, kp=kp: e.copy(out=XeT[:, kp * 2:kp * 2 + 2, :],
                                                                       in_=bank[:].rearrange("p (u c) -> p u c", u=2)),
                             reads=[bb], writes=[b_XeT])
                    for j in range(8):
                        ug, bug = load_u(wg_ap[:, :, j * 256:(j + 1) * 256])
                        ul, bul = load_u(wg_ap[:, :, 2048 + j * 256:2048 + (j + 1) * 256])
                        for ff in range(2):
                            f = j * 2 + ff
                            bank, bb = nb()
                            fns = [(lambda e, bank=bank, k=k, ff=ff, ug=ug: e.matmul(
                                bank[:, 0:CAP], lhsT=ug[:, k, ff * 128:(ff + 1) * 128], rhs=XeT[:, k, :],
                                start=(k == 0), stop=(k == 15))) for k in range(16)]
                            fns += [(lambda e, bank=bank, k=k, ff=ff, ul=ul: e.matmul(
                                bank[:, CAP:2 * CAP], lhsT=ul[:, k, ff * 128:(ff + 1) * 128], rhs=XeT[:, k, :],
                                start=(k == 0), stop=(k == 15))) for k in range(16)]
                            S.group("pe", fns, reads=[bug, bul, b_XeT], writes=[bb])
                            S.op("dve", lambda e, bank=bank, f=f, ex=ex: e.tensor_scalar(
                                out=gp[:], in0=bank[:, 0:CAP], scalar1=bgT[:, f, ex:ex + 1], scalar2=None, op0=ALU.add),
                                reads=[bb, b_bgT], writes=[b_gp])
                            S.op("dve", lambda e, bank=bank, f=f, ex=ex: e.tensor_scalar(
                                out=tl[:], in0=bank[:, CAP:2 * CAP], scalar1=bgT[:, 16 + f, ex:ex + 1], scalar2=SW_LIM,
                                op0=ALU.add, op1=ALU.min), reads=[bb, b_bgT], writes=[b_tl])
                            S.op("act", lambda e: e.activation(out=sg[:], in_=gp[:], func=AF.Exp, scale=-SW_ALPHA),
                                 reads=[b_gp], writes=[b_sg])
                            S.op("dve", lambda e: e.tensor_scalar(out=sg[:], in0=sg[:], scalar1=1.0, scalar2=None, op0=ALU.add),
                                 reads=[b_sg], writes=[b_sg])
                            S.op("dve", lambda e: e.reciprocal(out=sg[:], in_=sg[:]), reads=[b_sg], writes=[b_sg])
                            S.op("dve", lambda e: e.tensor_scalar(out=gt[:], in0=gp[:], scalar1=SW_LIM, scalar2=None, op0=ALU.min),
                                 reads=[b_gp], writes=[b_gt])
                            S.op("dve", lambda e: e.tensor_scalar(out=tl[:], in0=tl[:], scalar1=-SW_LIM, scalar2=1.0,
                                                                   op0=ALU.max, op1=ALU.add), reads=[b_tl], writes=[b_tl])
                            S.op("dve", lambda e: e.scalar_tensor_tensor(out=uu[:], in0=sg[:], scalar=SIG7, in1=gt[:],
                                                                         op0=ALU.min, op1=ALU.mult),
                                 reads=[b_sg, b_gt], writes=[b_uu])
                            S.op("dve", lambda e, f=f: e.tensor_tensor(out=hidT[:, f, :], in0=tl[:], in1=uu[:], op=ALU.mult),
                                 reads=[b_tl, b_uu], writes=[b_hidT])
                    for up in range(4):
                        banks = [nb() for _ in range(2)]
                        for u2 in range(2):
                            u = up * 2 + u2
                            ud, bud = load_u(wd_ap[:, :, u * 256:(u + 1) * 256])
                            for sb in range(2):
                                bank, bb = banks[sb]
                                S.group("pe", [
                                    (lambda e, bank=bank, f=f, sb=sb, u2=u2, ud=ud: e.matmul(
                                        bank[:, u2 * 256:(u2 + 1) * 256], lhsT=hidT[:, f, sb * 128:(sb + 1) * 128], rhs=ud[:, f, :],
                                        start=(f == 0), stop=(f == 15))) for f in range(16)],
                                    reads=[b_hidT, bud], writes=[bb])
                        for sb in range(2):
                            bank, bb = banks[sb]
                            S.op("act", lambda e, bank=bank, sb=sb, up=up: e.activation(
                                out=yw[:, sb, up * 512:(up + 1) * 512], in_=bank[:], func=AF.Identity, scale=wsl[:, sb:sb + 1]),
                                reads=[bb, b_wsl], writes=[b_yw])
                    for tt in range(8):
                        for n in range(4):
                            bank, bb = nb()
                            S.group("pe", [
                                (lambda e, bank=bank, sb=sb, tt=tt, n=n: e.matmul(
                                    bank[:], lhsT=SelT[:, sb, tt * 128:(tt + 1) * 128], rhs=yw[:, sb, n * 512:(n + 1) * 512],
                                    start=(sb == 0), stop=(sb == 1))) for sb in range(2)],
                                reads=[b_SelT, b_yw], writes=[bb])
                            S.op("dve", lambda e, bank=bank, n=n, tt=tt: e.tensor_tensor(
                                out=ACC[:, tt, n * 512:(n + 1) * 512], in0=bank[:], in1=ACC[:, tt, n * 512:(n + 1) * 512], op=ALU.add),
                                reads=[bb, b_ACC[tt]], writes=[b_ACC[tt]])
                S.barrier()
            with ExitStack() as es7:
                def A7(name, shape, dt):
                    return es7.enter_context(nc.sbuf_tensor(f"L{L}_{name}", list(shape), dt))

                g_bc = A7("g2_bc", [128, D], F32)
                be_bc = A7("be2_bc", [128, D], F32)
                b_gbc = Buf("gbc2")
                rowt = A7("rowt2", [1, D], F32)
                b_rowt = Buf("rowt2")
                bcast_row(g_bc, b_gbc, W["ln2_g"][L:L + 1, :], D, rowt, b_rowt)
                bcast_row(be_bc, b_gbc, W["ln2_b"][L:L + 1, :], D, rowt, b_rowt)
                st = A7("ln2_st", [128, 4, 6], F32)
                mv = A7("ln2_mv", [128, 2], F32)
                rstd = A7("ln2_rstd", [128, 1], F32)
                b_st = Buf("ln2st")
                if dbg == "ln2in":
                    for tt in range(8):
                        final_deps.append(S.dma("sp", lambda e, tt=tt: e.dma_start(
                            out=dbg_out[tt * 128:(tt + 1) * 128, :], in_=ACC[:, tt, :]), f"dbg{tt}", reads=[b_ACC[tt]]))
                    es7.close(); es4.close(); top_acc.close(); top_lg.close()
                    return
                for tt in range(8):
                    emit_ln((lambda c0, c1, tt=tt: ACC[:, tt, c0:c1]), b_ACC[tt], st, mv, rstd, b_st, g_bc, be_bc, b_gbc)
                    p = S.dma("sp", lambda e, tt=tt: e.dma_start(out=dst[tt * 128:(tt + 1) * 128, :], in_=ACC[:, tt, :]), "dst",
                              reads=[b_ACC[tt]])
                    if dst_is_out:
                        final_deps.append(p)
                    else:
                        b_dsts.readers.append(p)
                S.barrier()
        top_acc.close()
        top_lg.close()
        S.barrier()

    b_dsts = Buf("dsts")
    if len(layers) == 1:
        run_layer(layers[0], xo, xp, out, True)
    else:
        raise NotImplementedError("fused build added later")
    S.emit(final_deps)
    print("[kernel] instruction counts", S.n_inst, flush=True)
    return nc


_PROGS = {}


def _get_prog(layers):
    if layers not in _PROGS:
        _PROGS[layers] = build_program(layers)
    return _PROGS[layers]


def _core_maps(layer, src, inputs, n_exp=NE, moe=True, part=None, lg=None):
    maps = []
    f32 = lambda a: np.ascontiguousarray(a, dtype=np.float32)
    shared = {
        "router_w": f32(inputs["router_w"]), "router_b": f32(inputs["router_b"]),
        "ln1_g": f32(inputs["ln1_g"]), "ln1_b": f32(inputs["ln1_b"]),
        "ln2_g": f32(inputs["ln2_g"]), "ln2_b": f32(inputs["ln2_b"]),
    }
    if moe:
        ne = max(n_exp, 1)
        shared[f"w_gu{layer}"] = f32(inputs["moe_w_gate_up"][layer][:ne])
        shared[f"w_dn{layer}"] = f32(inputs["moe_w_down"][layer][:ne])
        shared["moe_b_gate_up"] = f32(inputs["moe_b_gate_up"])
        shared["moe_b_down"] = f32(inputs["moe_b_down"])
    if part == "C":
        pass
    elif layer == 0:
        shared["a_w_qkv"] = f32(inputs["a_w_qkv"][0])
        shared["a_w_o"] = f32(inputs["a_w_o"][0])
    else:
        shared["kv_w"] = f32(inputs["kv_w"])
        shared["b_w_q"] = f32(inputs["b_w_q"][0])
        shared["b_w_o"] = f32(inputs["b_w_o"][0])
    consts = [host_consts(0), host_consts(1)]
    for c in range(8):
        b, hf = c // 2, c % 2
        m = dict(shared)
        m["xo"] = f32(src[b, hf * T:(hf + 1) * T])
        m["xp"] = f32(src[b, 0:T])
        if lg is not None:
            m["lgin"] = f32(lg[c])
        m.update(consts[hf])
        maps.append(m)
    return maps


def _gather(res):
    o = np.zeros((4, 2048, D), np.float32)
    for c in range(8):
        b, hf = c // 2, c % 2
        o[b, hf * T:(hf + 1) * T] = res.results[c]["out"]
    return o


def _get_prog_part(layers, part):
    key = (layers, part)
    if key not in _PROGS:
        _PROGS[key] = build_program(layers, part=part)
    return _PROGS[key]


def kernel(**inputs):
    x = np.asarray(inputs["x"], dtype=np.float32)
    ids = list(range(8))
    r0 = run_bass_kernel_spmd(_get_prog((0,)), _core_maps(0, x, inputs), core_ids=ids)
    h1 = _gather(r0)
    ra = run_bass_kernel_spmd(_get_prog_part((1,), "AB"), _core_maps(1, h1, inputs, moe=False, part="AB"), core_ids=ids)
    hm = _gather(ra)
    lg = [ra.results[c]["lgout"] for c in range(8)]
    rc_ = run_bass_kernel_spmd(_get_prog_part((1,), "C"), _core_maps(1, hm, inputs, part="C", lg=lg), core_ids=ids)
    return _gather(rc_)
```
